# Optimizing a Trainium2 kernel written in Bass

```python
import math
import jax, jax.numpy as jnp
from jax import lax
import numpy as np

D_MODEL = 1024
BATCH = 8
SEQ = 2048
DEPTH = 2

GRID_W = 64
CTX_LEN = 256
WIN_H = 8
WIN_W = 16
HD_A = 64
H_A = D_MODEL // 2 // HD_A
H_B = 4
DV_B = D_MODEL // 2 // H_B
DK_B = DV_B // 2
MLSTM_CHUNK = 64
HD_C = 128
H_C = D_MODEL // HD_C
KV_C = max(H_C // 4, 1)
Q_BLOCK = 128
ROPE_THETA = 10000.0
N_EXPERTS = 16
N_GROUPS = 4
EXP_PER_GROUP = N_EXPERTS // N_GROUPS
TOP_K = 2
D_FF_EXPERT = D_MODEL // 2
N_EVEN = (DEPTH + 1) // 2
N_ODD = DEPTH // 2
D_MIX = H_A * HD_A + H_B * DV_B
SIZES_EVEN = (H_A * HD_A, H_A * HD_A, H_A * HD_A,
              H_B * DK_B, H_B * DK_B, H_B * DV_B, H_B * DV_B, 4 * H_B)
D_IN_EVEN = sum(SIZES_EVEN)
SIZES_ODD = (H_C * HD_C, KV_C * HD_C, KV_C * HD_C)
D_IN_ODD = sum(SIZES_ODD)
EPS = 1e-6
NEG_BIG = -1e30

kernel_name = "hybrid_natten_mlstm_gqa_moe_prefix"


def rms_norm(x, g):
    xf = x.astype(jnp.float32)
    y = xf * lax.rsqrt(jnp.mean(xf * xf, axis=-1, keepdims=True) + EPS)
    return (y * g.astype(jnp.float32)).astype(x.dtype)


def modulate(x, g, shift, scale):
    return rms_norm(x, g) * (1 + scale) + shift


def split_cols(p, sizes):
    points = [int(s) for s in np.cumsum(sizes)[:-1]]
    return jnp.split(p, points, axis=-1)


def rope_2d_tables(n_tokens, head_dim):
    n_freq = head_dim // 4
    inv_freq = ROPE_THETA ** (-jnp.arange(n_freq, dtype=jnp.float32) / n_freq)
    t = jnp.arange(n_tokens)
    rows = (t // GRID_W).astype(jnp.float32)
    cols = (t % GRID_W).astype(jnp.float32)
    ang = jnp.concatenate([rows[:, None] * inv_freq, cols[:, None] * inv_freq], axis=-1)
    return jnp.cos(ang), jnp.sin(ang)


def apply_rope(x, cos, sin):
    xr = x.astype(jnp.float32).reshape(*x.shape[:-1], -1, 2)
    x0, x1 = xr[..., 0], xr[..., 1]
    c = cos[:, None, :]
    s = sin[:, None, :]
    out = jnp.stack([x0 * c - x1 * s, x0 * s + x1 * c], axis=-1).reshape(x.shape)
    return out.astype(x.dtype)


def block_attention(q, k, v):
    B, S, H, d = q.shape
    kv = k.shape[2]
    G = H // kv
    qb = q.reshape(B, S // Q_BLOCK, Q_BLOCK, kv, G, d).transpose(1, 0, 2, 3, 4, 5)

    def one_block(qi):
        s = jnp.einsum('bqkgd,btkd->bkgqt', qi, k).astype(jnp.float32)
        p = jax.nn.softmax(s, axis=-1).astype(v.dtype)
        return jnp.einsum('bkgqt,btkd->bqkgd', p, v)

    o = lax.map(one_block, qb)
    return o.transpose(1, 0, 2, 3, 4, 5).reshape(B, S, H * d)


def neighbourhood_attention(q, k, v, kc, vc, rpb):
    B, S, H, d = q.shape
    rows = S // GRID_W
    kh = min(WIN_H, rows)
    n_loc = kh * WIN_W
    qg = q.reshape(B, rows, GRID_W, H, d)
    kg = k.reshape(B, rows, GRID_W, H, d)
    vg = v.reshape(B, rows, GRID_W, H, d)
    qcol = np.arange(GRID_W)
    col_start = np.clip(qcol - WIN_W // 2, 0, GRID_W - WIN_W)
    col_idx = col_start[:, None] + np.arange(WIN_W)[None, :]
    dcol = col_idx - qcol[:, None] + (WIN_W - 1)

    def row_block(r):
        r0 = jnp.clip(r - kh // 2, 0, rows - kh)
        q_r = lax.dynamic_index_in_dim(qg, r, axis=1, keepdims=False)
        k_r = lax.dynamic_slice_in_dim(kg, r0, kh, axis=1)
        v_r = lax.dynamic_slice_in_dim(vg, r0, kh, axis=1)
        k_win = k_r[:, :, col_idx]
        v_win = v_r[:, :, col_idx]
        s_loc = jnp.einsum('bqhd,biqjhd->bhqij', q_r, k_win)
        drow = r0 + jnp.arange(kh) - r + (WIN_H - 1)
        bias = rpb[:, drow][:, :, dcol].transpose(0, 2, 1, 3)
        s_loc = s_loc + bias[None].astype(s_loc.dtype)
        s_ctx = jnp.einsum('bqhd,blhd->bhql', q_r, kc)
        s = jnp.concatenate([s_loc.reshape(B, H, GRID_W, n_loc), s_ctx], axis=-1)
        p = jax.nn.softmax(s.astype(jnp.float32), axis=-1).astype(v.dtype)
        p_loc = p[..., :n_loc].reshape(B, H, GRID_W, kh, WIN_W)
        o = jnp.einsum('bhqij,biqjhd->bqhd', p_loc, v_win) + jnp.einsum('bhql,blhd->bqhd', p[..., n_loc:], vc)
        return o

    o = lax.map(row_block, jnp.arange(rows))
    return o.transpose(1, 0, 2, 3, 4).reshape(B, S, H * d)


def mlstm_chunkwise(q, k, v, ig, lf):
    B, H, T, dk = q.shape
    dv = v.shape[-1]
    n_ch = T // MLSTM_CHUNK

    def to_chunks(a):
        return jnp.moveaxis(a.reshape(B, H, n_ch, MLSTM_CHUNK, *a.shape[3:]), 2, 0)

    causal = jnp.tril(jnp.ones((MLSTM_CHUNK, MLSTM_CHUNK), dtype=bool))

    def step(carry, inp):
        C, n, m = carry
        qj, kj, vj, ij, fj = inp
        b = jnp.cumsum(fj, axis=-1)
        a = b + m[..., None]
        Dm = jnp.where(causal, b[..., :, None] - b[..., None, :] + ij[..., None, :], NEG_BIG)
        m_row = jnp.maximum(a, Dm.max(axis=-1))
        w_intra = jnp.exp(Dm - m_row[..., None])
        w_inter = jnp.exp(a - m_row)
        qk = jnp.einsum('bhid,bhjd->bhij', qj, kj) * w_intra
        num = w_inter[..., None] * jnp.einsum('bhvd,bhid->bhiv', C, qj) + jnp.einsum('bhij,bhjv->bhiv', qk, vj)
        den = w_inter * jnp.einsum('bhd,bhid->bhi', n, qj) + qk.sum(axis=-1)
        h = num / jnp.maximum(jnp.abs(den), jnp.exp(-m_row))[..., None]
        bL = b[..., -1]
        g = bL[..., None] - b + ij
        m_new = jnp.maximum(bL + m, g.max(axis=-1))
        w_old = jnp.exp(bL + m - m_new)
        w_tok = jnp.exp(g - m_new[..., None])
        C_new = w_old[..., None, None] * C + jnp.einsum('bhj,bhjv,bhjd->bhvd', w_tok, vj, kj)
        n_new = w_old[..., None] * n + jnp.einsum('bhj,bhjd->bhd', w_tok, kj)
        return (C_new, n_new, m_new), h

    init = (jnp.zeros((B, H, dv, dk), jnp.float32), jnp.zeros((B, H, dk), jnp.float32),
            jnp.full((B, H), NEG_BIG, jnp.float32))
    _, hs = lax.scan(step, init, (to_chunks(q), to_chunks(k), to_chunks(v), to_chunks(ig), to_chunks(lf)))
    return jnp.moveaxis(hs, 0, 2).reshape(B, H, T, dv)


def even_mixer(h_lat, h_ctx, w_in, w_out, na_qg, na_kg, rpb, gate_b, ml_norm_g, need_ctx_out):
    B, S, _ = h_lat.shape
    L = h_ctx.shape[1]
    aq, ak, av, mq, mk, mv, mo, mg = split_cols(h_lat @ w_in, SIZES_EVEN)
    caq, cak, cav, cmq, cmk, cmv, cmo, cmg = split_cols(h_ctx @ w_in, SIZES_EVEN)

    def heads_a(t):
        return t.reshape(B, -1, H_A, HD_A)
    q_a = rms_norm(heads_a(aq), na_qg) * HD_A ** -0.5
    k_a = rms_norm(heads_a(ak), na_kg)
    ck_a = rms_norm(heads_a(cak), na_kg)
    cv_a = heads_a(cav)
    o_a = neighbourhood_attention(q_a, k_a, heads_a(av), ck_a, cv_a, rpb)

    def heads_b(t, dh):
        return jnp.moveaxis(t.reshape(B, -1, H_B, dh), 2, 1).astype(jnp.float32)
    q_m = jnp.concatenate([heads_b(cmq, DK_B), heads_b(mq, DK_B)], axis=2) * DK_B ** -0.5
    k_m = jnp.concatenate([heads_b(cmk, DK_B), heads_b(mk, DK_B)], axis=2)
    v_m = jnp.concatenate([heads_b(cmv, DV_B), heads_b(mv, DV_B)], axis=2)
    gates = jnp.concatenate([cmg, mg], axis=1).astype(jnp.float32) + gate_b.astype(jnp.float32)
    i_f, f_f, i_b, f_b = gates.reshape(B, L + S, 4, H_B).transpose(2, 0, 3, 1)

    def flip(t):
        return jnp.concatenate([t[:, :, :L][:, :, ::-1], t[:, :, L:][:, :, ::-1]], axis=2)

    h_f = mlstm_chunkwise(q_m, k_m, v_m, i_f, jax.nn.log_sigmoid(f_f))
    h_bw = flip(mlstm_chunkwise(flip(q_m), flip(k_m), flip(v_m), flip(i_b), flip(jax.nn.log_sigmoid(f_b))))
    h_m = jnp.moveaxis(h_f + h_bw, 1, 2)
    h_m = rms_norm(h_m, ml_norm_g.reshape(H_B, DV_B)).reshape(B, L + S, H_B * DV_B).astype(h_lat.dtype)
    h_m = h_m * jax.nn.sigmoid(jnp.concatenate([cmo, mo], axis=1))

    out_lat = jnp.concatenate([o_a, h_m[:, L:]], axis=-1) @ w_out
    if not need_ctx_out:
        return out_lat, None
    cq_a = rms_norm(heads_a(caq), na_qg) * HD_A ** -0.5
    co_a = block_attention(cq_a, ck_a, cv_a)
    out_ctx = jnp.concatenate([co_a, h_m[:, :L]], axis=-1) @ w_out
    return out_lat, out_ctx


def odd_mixer(h_lat, h_ctx, w_in, w_out, qg, kg, cos, sin, need_ctx_out):
    B, S, _ = h_lat.shape
    L = h_ctx.shape[1]
    q, k, v = split_cols(h_lat @ w_in, SIZES_ODD)
    q = apply_rope(rms_norm(q.reshape(B, S, H_C, HD_C), qg), cos, sin) * HD_C ** -0.5
    k = apply_rope(rms_norm(k.reshape(B, S, KV_C, HD_C), kg), cos, sin)
    v = v.reshape(B, S, KV_C, HD_C)
    if need_ctx_out:
        cq, ck, cv = split_cols(h_ctx @ w_in, SIZES_ODD)
    else:
        ck, cv = split_cols(h_ctx @ w_in[:, H_C * HD_C:], SIZES_ODD[1:])
    ck = rms_norm(ck.reshape(B, L, KV_C, HD_C), kg)
    cv = cv.reshape(B, L, KV_C, HD_C)
    k_all = jnp.concatenate([ck, k], axis=1)
    v_all = jnp.concatenate([cv, v], axis=1)
    out_lat = block_attention(q, k_all, v_all) @ w_out
    if not need_ctx_out:
        return out_lat, None
    cq = rms_norm(cq.reshape(B, L, H_C, HD_C), qg) * HD_C ** -0.5
    out_ctx = block_attention(cq, ck, cv) @ w_out
    return out_lat, out_ctx


def moe(h, router_w, router_b, w1, w3, w2):
    N = h.shape[0]
    scores = jax.nn.sigmoid((h @ router_w).astype(jnp.float32))
    sel = (scores + router_b.astype(jnp.float32)).reshape(N, N_GROUPS, EXP_PER_GROUP)
    group_score = lax.top_k(sel, TOP_K)[0].sum(axis=-1)
    g_idx = jnp.argmax(group_score, axis=-1)
    sel_in = jnp.take_along_axis(sel, g_idx[:, None, None], axis=1)[:, 0]
    _, local = lax.top_k(sel_in, TOP_K)
    e_idx = g_idx[:, None] * EXP_PER_GROUP + local
    w = jnp.take_along_axis(scores, e_idx, axis=-1)
    w = w / w.sum(axis=-1, keepdims=True)
    comb = (jax.nn.one_hot(e_idx, N_EXPERTS, dtype=jnp.float32) * w[..., None]).sum(axis=1).astype(h.dtype)
    y = jnp.zeros_like(h)
    for e in range(N_EXPERTS):
        y = y + comb[:, e:e + 1] * ((jax.nn.silu(h @ w1[e]) * (h @ w3[e])) @ w2[e])
    return y


def setup_inputs(seed: int = 0) -> dict:
    key = jax.random.key(seed)
    ks = jax.random.split(key, 32)
    D = D_MODEL

    def nrm(k, shape, scale):
        return jax.random.normal(k, shape, jnp.float32) * scale

    i_b = nrm(ks[13], (N_EVEN, 2, H_B), 0.1)
    f_b = 3.0 + 3.0 * jax.random.uniform(ks[14], (N_EVEN, 2, H_B), jnp.float32)
    mlstm_gate_b = jnp.stack([i_b[:, 0], f_b[:, 0], i_b[:, 1], f_b[:, 1]], axis=1).reshape(N_EVEN, 4 * H_B)
    return {
        "x": nrm(ks[0], (BATCH, SEQ, D), 1.0),
        "c": nrm(ks[1], (BATCH, D), 1.0),
        "ctx": nrm(ks[2], (BATCH, CTX_LEN, D), 1.0),
        "c_ctx": nrm(ks[3], (D,), 1.0),
        "ada_w": nrm(ks[4], (DEPTH, D, 6 * D), 0.5 * D ** -0.5),
        "ada_b": nrm(ks[5], (DEPTH, 6 * D), 0.02),
        "norm_mix_g": 1.0 + nrm(ks[6], (DEPTH, D), 0.05),
        "norm_ffn_g": 1.0 + nrm(ks[7], (DEPTH, D), 0.05),
        "even_w_in": nrm(ks[8], (N_EVEN, D, D_IN_EVEN), D ** -0.5),
        "even_w_out": nrm(ks[9], (N_EVEN, D_MIX, D), D_MIX ** -0.5),
        "na_q_norm_g": 1.0 + nrm(ks[10], (N_EVEN, HD_A), 0.05),
        "na_k_norm_g": 1.0 + nrm(ks[11], (N_EVEN, HD_A), 0.05),
        "na_rpb": nrm(ks[12], (N_EVEN, H_A, 2 * WIN_H - 1, 2 * WIN_W - 1), 0.02),
        "mlstm_gate_b": mlstm_gate_b,
        "mlstm_norm_g": 1.0 + nrm(ks[15], (N_EVEN, H_B * DV_B), 0.05),
        "odd_w_in": nrm(ks[16], (N_ODD, D, D_IN_ODD), D ** -0.5),
        "odd_w_out": nrm(ks[17], (N_ODD, H_C * HD_C, D), (H_C * HD_C) ** -0.5),
        "gqa_q_norm_g": 1.0 + nrm(ks[18], (N_ODD, HD_C), 0.05),
        "gqa_k_norm_g": 1.0 + nrm(ks[19], (N_ODD, HD_C), 0.05),
        "router_w": nrm(ks[20], (D, N_EXPERTS), D ** -0.5),
        "router_b": nrm(ks[21], (N_EXPERTS,), 0.01),
        "exp_w1": nrm(ks[22], (DEPTH, N_EXPERTS, D, D_FF_EXPERT), D ** -0.5),
        "exp_w3": nrm(ks[23], (DEPTH, N_EXPERTS, D, D_FF_EXPERT), D ** -0.5),
        "exp_w2": nrm(ks[24], (DEPTH, N_EXPERTS, D_FF_EXPERT, D), D_FF_EXPERT ** -0.5),
    }


def reference(x, c, ctx, c_ctx, ada_w, ada_b, norm_mix_g, norm_ffn_g, even_w_in, even_w_out,
              na_q_norm_g, na_k_norm_g, na_rpb, mlstm_gate_b, mlstm_norm_g, odd_w_in, odd_w_out,
              gqa_q_norm_g, gqa_k_norm_g, router_w, router_b, exp_w1, exp_w3, exp_w2):
    B, S, D = x.shape
    L = ctx.shape[1]
    cos, sin = rope_2d_tables(S, HD_C)
    silu_c = jax.nn.silu(c)
    silu_cc = jax.nn.silu(c_ctx)
    x_lat, x_ctx = x, ctx
    for l in range(DEPTH):
        last = l == DEPTH - 1
        mod_lat = (silu_c @ ada_w[l] + ada_b[l])[:, None, :]
        mod_ctx = (silu_cc @ ada_w[l] + ada_b[l])[None, None, :]
        sh1, sc1, g1, sh2, sc2, g2 = jnp.split(mod_lat, 6, axis=-1)
        csh1, csc1, cg1, csh2, csc2, cg2 = jnp.split(mod_ctx, 6, axis=-1)

        h_lat = modulate(x_lat, norm_mix_g[l], sh1, sc1)
        h_ctx = modulate(x_ctx, norm_mix_g[l], csh1, csc1)
        if l % 2 == 0:
            i = l // 2
            o_lat, o_ctx = even_mixer(h_lat, h_ctx, even_w_in[i], even_w_out[i], na_q_norm_g[i], na_k_norm_g[i],
                                      na_rpb[i], mlstm_gate_b[i], mlstm_norm_g[i], not last)
        else:
            i = l // 2
            o_lat, o_ctx = odd_mixer(h_lat, h_ctx, odd_w_in[i], odd_w_out[i], gqa_q_norm_g[i], gqa_k_norm_g[i],
                                     cos, sin, not last)
        x_lat = x_lat + g1 * o_lat

        h_lat = modulate(x_lat, norm_ffn_g[l], sh2, sc2).reshape(B * S, D)
        if last:
            y_lat = moe(h_lat, router_w, router_b, exp_w1[l], exp_w3[l], exp_w2[l])
        else:
            x_ctx = x_ctx + cg1 * o_ctx
            h_ctx = modulate(x_ctx, norm_ffn_g[l], csh2, csc2).reshape(B * L, D)
            y = moe(jnp.concatenate([h_lat, h_ctx], axis=0), router_w, router_b, exp_w1[l], exp_w3[l], exp_w2[l])
            y_lat = y[:B * S]
            x_ctx = x_ctx + cg2 * y[B * S:].reshape(B, L, D)
        x_lat = x_lat + g2 * y_lat.reshape(B, S, D)
    return x_lat
```

```python
import contextlib
import os
import numpy as np
import concourse.bass as bass
import concourse.mybir as mybir
from concourse.bass_utils import run_bass_kernel_spmd

F32 = mybir.dt.float32
BF16 = mybir.dt.bfloat16
AF = mybir.ActivationFunctionType
ALU = mybir.AluOpType
AX = mybir.AxisListType

D = 1024
L_CTX = 256
S_LAT = 2048
T = L_CTX + S_LAT
NT = T // 128
KC = D // 128
EPS = 1e-6
NE = 16
DFF = 512
NEG = -30000.0
TT = [(0, 256), (256, 512), (768, 512), (1280, 512), (1792, 512)]

ENGS = ("pe", "act", "dve", "pool", "sp")


class Instr:
    __slots__ = ("eng", "fn", "dma", "deps", "signal", "sig", "sem", "target")

    def __init__(self, eng, fn, dma):
        self.eng = eng
        self.fn = fn
        self.dma = dma
        self.deps = ()
        self.signal = False
        self.sig = 0
        self.sem = None
        self.target = 0


class Prog:
    def __init__(self):
        self.instrs = {e: [] for e in ENGS}
        self.wr = {}
        self.wr_dma = {}
        self.rd = {}
        self.rd_dma = {}
        self.pending = {e: [] for e in ENGS}

    def fence(self):
        lasts = [self.instrs[e][-1] for e in ("pe", "act", "dve", "pool") if self.instrs[e]]
        lasts += [i for i in self.instrs["sp"] if i.dma][-24:]
        for e in ENGS:
            self.pending[e] = list(lasts)

    def add(self, eng, fn, reads=(), writes=(), dma=False):
        ins = Instr(eng, fn, dma)
        pr = [k for k in reads if k[0] in ("ps", "psb")]
        if pr:
            reads = [k for k in reads if k[0] not in ("ps", "psb")]
            writes = list(writes) + pr
        deps = {}
        for k in reads:
            w = self.wr.get(k)
            if w:
                for d in w.values():
                    deps[id(d)] = d
            d = self.wr_dma.get(k)
            if d is not None:
                deps[id(d)] = d
        for k in writes:
            w = self.wr.get(k)
            if w:
                for d in w.values():
                    deps[id(d)] = d
            d = self.wr_dma.get(k)
            if d is not None:
                deps[id(d)] = d
            r = self.rd.get(k)
            if r:
                for d in r.values():
                    deps[id(d)] = d
            for d in self.rd_dma.get(k, ()):
                deps[id(d)] = d
        for d in self.pending[eng]:
            deps[id(d)] = d
        self.pending[eng] = []
        ins.deps = [d for d in deps.values() if d is not ins and (d.dma or d.eng != eng or dma or eng != "pe")]
        for d in ins.deps:
            d.signal = True
        for k in reads:
            if dma:
                self.rd_dma.setdefault(k, []).append(ins)
            else:
                self.rd.setdefault(k, {})[eng] = ins
        for k in writes:
            if dma:
                self.wr_dma[k] = ins
                self.wr[k] = {}
            else:
                self.wr.setdefault(k, {})[eng] = ins
                self.wr_dma[k] = None
            self.rd[k] = {}
            self.rd_dma[k] = []
        self.instrs[eng].append(ins)
        return ins

    def emit(self, nc, es, n_dma_sems=24):
        handles = {"pe": nc.tensor, "act": nc.scalar, "dve": nc.vector, "pool": nc.gpsimd, "sp": nc.sync}
        esem = {e: es.enter_context(nc.semaphore("c_" + e)) for e in ENGS}
        dsems = {e: [es.enter_context(nc.semaphore("d_%s%d" % (e, i))) for i in range(n_dma_sems)]
                 for e in ("sp",)}
        prev_dma = {}
        for e in ENGS:
            idx = 0
            nd = 0
            for ins in self.instrs[e]:
                if ins.dma:
                    pool = dsems[e]
                    ins.sem = pool[nd % len(pool)]
                    ins.target = 16 * (nd // len(pool) + 1)
                    nd += 1
                elif ins.signal:
                    idx += 1
                    ins.sig = idx
        block = es.enter_context(nc.Block())

        def body(e):
            def run(eng):
                waited = {}
                for ins in self.instrs[e]:
                    for d in ins.deps:
                        if d.dma:
                            sem, val = d.sem, d.target
                        else:
                            sem, val = esem[d.eng], d.sig
                        key = id(sem)
                        if waited.get(key, 0) >= val:
                            continue
                        eng.wait_ge(sem, val)
                        waited[key] = val
                    if ins.dma and ins.target > 16:
                        key = id(ins.sem)
                        if waited.get(key, 0) < ins.target - 16:
                            eng.wait_ge(ins.sem, ins.target - 16)
                            waited[key] = ins.target - 16
                    r = ins.fn(eng)
                    if ins.dma:
                        r.then_inc(ins.sem, 16)
                    elif ins.signal:
                        r.then_inc(esem[e], 1)
            return run

        block.sync(body("sp"))
        block.tensor(body("pe"))
        block.scalar(body("act"))
        block.vector(body("dve"))
        block.gpsimd(body("pool"))


def bkeys(name, t0, n):
    return [(name, b) for b in range(t0 // 128, (t0 + n + 127) // 128)]


class K:
    pass


def build_program(stop=None, debug=0):
    nc = bass.Bass("TRN2", target_bir_lowering=False)
    P = Prog()
    es = contextlib.ExitStack()
    es.__enter__()

    def dram_in(name, shape, dt=F32):
        return nc.dram_tensor(name, list(shape), dt, kind="ExternalInput").ap()

    xT = dram_in("xT", [D, T])
    cvec = dram_in("cvec", [128, KC, 2])
    ada_w = dram_in("ada_w", [2, D, 6 * D])
    adab = dram_in("adab", [128, 2, 48])
    nrm_g = dram_in("nrm_g", [128, 2, 2, KC])
    w_in0 = dram_in("w_in0", [D, 3088])
    w_out0 = dram_in("w_out0", [D, D])
    wgate = dram_in("wgate", [D, 128])
    gateb = dram_in("gateb", [128, 2])
    na_g = dram_in("na_g", [128, 2])
    btab = dram_in("btab", [8, 20, 128, 512])
    ml_g = dram_in("ml_g", [128, 512])
    w_in1 = dram_in("w_in1", [D, 1536])
    w_out1 = dram_in("w_out1", [D, D])
    gqa_g = dram_in("gqa_g", [128, 2])
    ropeC = dram_in("ropeC", [128, S_LAT])
    ropeS = dram_in("ropeS", [128, S_LAT])
    router_w = dram_in("router_w", [D, NE])
    router_b = dram_in("router_b", [128, NE])
    exp_w1 = dram_in("exp_w1", [2, NE, D, DFF])
    exp_w3 = dram_in("exp_w3", [2, NE, D, DFF])
    exp_w2 = dram_in("exp_w2", [2, NE, DFF, D])
    consts = dram_in("consts", [128, 1024])
    outT = nc.dram_tensor("outT", [D, S_LAT], F32, kind="ExternalOutput").ap()
    xsp = nc.dram_tensor("xsp", [D, T], F32, kind="Internal").ap()
    dbg = None
    if debug:
        dbg = nc.dram_tensor("dbg", [128, debug], F32, kind="ExternalOutput").ap()

    def sb(name, shape, dt):
        return es.enter_context(nc.sbuf_tensor(name, list(shape), dt))

    XR = sb("XR", [128, KC * T], F32)
    HT = sb("HT", [128, KC, T], BF16)
    MIX = sb("MIX", [128, KC * T], BF16)
    SCR = sb("SCR", [128, 9216], F32)
    CST = sb("CST", [128, 1024], F32)
    CSTB = sb("CSTB", [128, 1024], BF16)
    MOD = sb("MOD", [128, 2, 48, 2], F32)
    GS = sb("GS", [128, 2, 2, KC, 2], F32)
    SML = sb("SML", [128, 256], F32)
    PS = [es.enter_context(nc.psum_tensor("ps%d" % i, [128, 512], F32)) for i in range(7)]
    PSB = es.enter_context(nc.psum_tensor("psb", [128, 1024], BF16))

    X = XR[:].rearrange("p (k t) -> p k t", k=KC)
    k_ = K()
    k_.nc, k_.P = nc, P

    ident = CST[:, 0:128]
    ones_f = CST[:, 128:256]
    identb = CSTB[:, 0:128]
    bd64b = CSTB[:, 768:896]
    trif = CST[:, 256:384]
    trib = CST[:, 384:512]
    swapb = CSTB[:, 512:640]
    onesb = CSTB[:, 640:768]

    def dma(out, in_, reads, writes, eng="sp"):
        return P.add(eng, lambda e: e.dma_start(out=out, in_=in_), reads, writes, dma=True)

    def mm(out, lhsT, rhs, start, stop, reads, writes):
        return P.add("pe", lambda e: e.matmul(out, lhsT=lhsT, rhs=rhs, start=start, stop=stop), reads, writes)

    def tr(out, in_, idn, reads, writes):
        return P.add("pe", lambda e: e.transpose(out=out, in_=in_, identity=idn), reads, writes)

    def act(out, in_, func, reads, writes, scale=None, bias=None):
        kw = {}
        if scale is not None:
            kw["scale"] = scale
        if bias is not None:
            kw["bias"] = bias
        return P.add("act", lambda e: e.activation(out=out, in_=in_, func=func, **kw), reads, writes)

    def tt(out, in0, in1, op, reads, writes, eng="dve"):
        return P.add(eng, lambda e: e.tensor_tensor(out=out, in0=in0, in1=in1, op=op), reads, writes)

    def ts(out, in0, s1, s2, op0, op1, reads, writes, eng="dve"):
        if op1 is None:
            return P.add(eng, lambda e: e.tensor_scalar(out=out, in0=in0, scalar1=s1, scalar2=None, op0=op0), reads, writes)
        return P.add(eng, lambda e: e.tensor_scalar(out=out, in0=in0, scalar1=s1, scalar2=s2, op0=op0, op1=op1), reads, writes)

    def stt(out, in0, scalar, in1, op0, op1, reads, writes):
        return P.add("dve", lambda e: e.scalar_tensor_tensor(out=out, in0=in0, scalar=scalar, in1=in1, op0=op0, op1=op1),
                     reads, writes)

    def cp(out, in_, reads, writes, eng="dve"):
        if eng == "act":
            return P.add(eng, lambda e: e.activation(out=out, in_=in_, func=AF.Copy), reads, writes)
        return P.add(eng, lambda e: e.tensor_copy(out=out, in_=in_), reads, writes)

    def memset(ap, val, writes, eng="pool"):
        return P.add(eng, lambda e: e.memset(ap, val), (), writes)

    def red(out, in_, op, reads, writes, axis=AX.X):
        return P.add("dve", lambda e: e.tensor_reduce(out=out, in_=in_, axis=axis, op=op), reads, writes)

    def recip(out, in_, reads, writes):
        return P.add("dve", lambda e: e.reciprocal(out=out, in_=in_), reads, writes)

    dbg_off = [0]
    cnt = {"nm": 0, "ada": 0}

    def tap(ap, n, reads):
        if dbg is None or debug == 1:
            return
        o = dbg_off[0]
        dbg_off[0] += n
        k_.dbg_slots.append((o, n))
        stg = k_.dbg_stage
        cp(stg[:, 0:n], ap, reads + [("dbgstage",)], [("dbgstage",)])
        dma(dbg[:, o:o + n], stg[:, 0:n], [("dbgstage",)], [("dbg", o)])
        k_.out_keys.append(("dbg", o))

    k_.dbg_slots = []
    k_.out_keys = []
    k_.dbg_stage = None
    if dbg is not None and debug > 1:
        k_.dbg_stage = sb("dbgst", [128, 2304], F32)

    def finish():
        P.add("sp", lambda e: e.nop(), list(k_.out_keys), [("final",)])
        P.emit(nc, es)
        es.close()
        return nc, k_

    dma(CST[:], consts[:, :], [], [("CST",)])
    cp(CSTB[:], CST[:], [("CST",)], [("CSTB",)], eng="dve")
    dma(SML[:, 0:2], gateb[:, :], [], [("gateb",)])
    dma(SML[:, 2:4], na_g[:, :], [], [("na_g",)])
    dma(SML[:, 4:6], gqa_g[:, :], [], [("gqa_g",)])
    dma(SML[:, 16:32], router_b[:, :], [], [("router_b",)])
    NRM = sb("NRM", [128, 2, 2, KC], F32)
    dma(NRM[:], nrm_g[:, :, :, :], [], [("NRM",)])
    ADB = sb("ADB", [128, 2, 48], F32)
    dma(ADB[:], adab[:, :, :], [], [("ADB",)])
    CV = sb("CV", [128, KC, 2], F32)
    dma(CV[:], cvec[:, :, :], [], [("CV",)])
    SCV = sb("SCV", [128, KC, 2], F32)
    act(SCV[:], CV[:], AF.Silu, [("CV",)], [("SCV",)])

    def ada_gs(l, w):
        base = 8 if w == 0 else 32
        ts(GS[:, l, w], MOD[:, l, base:base + 8, :], 1.0, None, ALU.add, None, [("MOD", l)], [("GS", l, w)])
        tt(GS[:, l, w], GS[:, l, w], NRM[:, l, w].unsqueeze(2).to_broadcast([128, KC, 2]), ALU.mult,
           [("GS", l, w), ("NRM",)], [("GS", l, w)])

    def ada_block(l, j0, nch, stage, skey, ps, pkey):
        aw = ada_w[l].rearrange("(k p) c -> p k c", p=128)
        dma(stage, aw[:, :, j0 * 128:(j0 + nch) * 128], [], [skey])
        for jj in range(nch):
            for kc in range(KC):
                mm(ps[:, 2 * jj:2 * jj + 2], stage[:, kc, jj * 128:(jj + 1) * 128], SCV[:, kc, :],
                   kc == 0, kc == KC - 1, [skey, ("SCV",)], [pkey])
        tt(MOD[:, l, j0:j0 + nch, :], ps[:, 0:2 * nch].rearrange("p (j t) -> p j t", t=2),
           ADB[:, l, j0:j0 + nch].unsqueeze(2).to_broadcast([128, nch, 2]), ALU.add, [pkey, ("ADB",)], [("MOD", l)])

    stage0 = XR[:, 0:8192].rearrange("p (s k c) -> p s k c", s=2, k=KC)
    for blk in range(4):
        ada_block(0, blk * 4, 4, stage0[:, blk % 2], ("adast", blk % 2), PS[6], ("ps", 6))
    ada_gs(0, 0)
    ada_list = [(0, j) for j in range(16, 48)] + [(1, j) for j in range(48)]
    PSBf = PSB[:].bitcast(F32)

    SCVb = sb("SCVb", [128, KC, 2], BF16)
    cp(SCVb[:], SCV[:], [("SCV",)], [("SCVb",)])
    MT = sb("MT", [2, 256], F32)

    ABW4 = MIX[:, 9216:13312].rearrange("p (s k c) -> p s k c", s=4, k=KC)
    issued = [0]

    def ada_bg_dma(upto):
        while issued[0] < min(upto, len(ada_list)):
            idx = issued[0]
            issued[0] += 1
            l_, j = ada_list[idx]
            k = idx % 4
            stg = SCR[:, 4608 + k * 1024:4608 + (k + 1) * 1024].rearrange("p (k c) -> p k c", k=KC)
            aw = ada_w[l_].rearrange("(k p) c -> p k c", p=128)
            dma(stg, aw[:, :, j * 128:(j + 1) * 128], [], [("adabg", k)])
            cp(ABW4[:, k], stg, [("adabg", k)], [("abw", k)], eng="pool")

    def ada_bg():
        idx = cnt["ada"]
        if idx >= len(ada_list):
            return
        cnt["ada"] += 1
        ada_bg_dma(idx + 4)
        l_, j = ada_list[idx]
        k = idx % 4
        c2 = 2 * (idx % 2)
        for kc in range(KC):
            mm(PSBf[:, c2:c2 + 2], ABW4[:, k, kc, :], SCVb[:, kc, :], kc == 0, kc == KC - 1, [("SCVb",), ("abw", k)], [("psb",)])
        tt(MOD[:, l_, j, :], PSBf[:, c2:c2 + 2], ADB[:, l_, j:j + 1].to_broadcast([128, 2]), ALU.add, [("psb",), ("ADB",)], [("MOD", l_)])

    def ada_finish():
        while cnt["ada"] < len(ada_list):
            ada_bg()
        ada_gs(0, 1)
        ada_gs(1, 0)
        ada_gs(1, 1)

    if stop == "ada":
        ada_finish()
        tap(MOD[:, 0].rearrange("p j t -> p (j t)"), 96, [("MOD", 0)])
        tap(MOD[:, 1].rearrange("p j t -> p (j t)"), 96, [("MOD", 1)])
        return finish()

    def norm_modulate(l, w, src_tile, tiles, h32_cb=None):
        sh_base = 0 if w == 0 else 24
        SCRb_ = SCR[:].bitcast(BF16)
        for i in tiles:
            t0, n = TT[i]
            seg = 1 if i == 0 else 0
            xa, xk = src_tile(i)
            s2 = cnt["nm"] % 2
            cnt["nm"] += 1
            sq = SCR[:, s2 * 4096:(s2 + 1) * 4096].rearrange("p (k t) -> p k t", k=KC)[:, :, 0:n]
            sqb = SCRb_[:, 16384 + s2 * 1024:16384 + (s2 + 1) * 1024]
            sqb = SCRb_[:, s2 * 8192:s2 * 8192 + 4096].rearrange("p (k t) -> p k t", k=KC)[:, :, 0:n]
            act(sqb, xa, AF.Square, xk + [("sq", s2)], [("sq", s2)])
            ps = PS[5]
            for kc in range(KC):
                mm(ps[:, 0:n], onesb, sqb[:, kc, :], kc == 0, kc == KC - 1, [("sq", s2), ("CSTB",)], [("ps", 5)])
            rstd = SCR[:, 8192 + s2 * 512:8192 + (s2 + 1) * 512][:, 0:n]
            act(rstd, ps[:, 0:n], AF.Ln, [("ps", 5)], [("rstd", s2)], scale=1.0 / D, bias=EPS)
            act(rstd, rstd, AF.Exp, [("rstd", s2)], [("rstd", s2)], scale=-0.5)
            tt(sq, xa, rstd.unsqueeze(1).to_broadcast([128, KC, n]), ALU.mult, xk + [("rstd", s2), ("sq", s2)], [("sq", s2)])
            for kc in range(KC):
                ts(HT[:, kc, t0:t0 + n], sq[:, kc, :], GS[:, l, w, kc, seg:seg + 1], MOD[:, l, sh_base + kc, seg:seg + 1],
                   ALU.mult, ALU.add, [("sq", s2), ("GS", l, w), ("MOD", l)], bkeys("HT", t0, n))
            if h32_cb is not None:
                h32_cb(i, sq, s2, t0, n)

    xv = xT.rearrange("(k p) t -> p k t", p=128)
    XST = XR[:, 8192:16384].rearrange("p (s k t) -> p s k t", s=2, k=KC)

    def src_dram(view):
        def f(i):
            t0, n = TT[i]
            s = i % 2
            dma(XST[:, s, :, 0:n], view[:, :, t0:t0 + n], [], [("xst", s)])
            return XST[:, s, :, 0:n], [("xst", s)]
        return f

    norm_modulate(0, 0, src_dram(xv), range(5))
    if stop == "h0dbg":
        return finish()
    if stop == "h0":
        for kc in range(KC):
            tap(HT[:, kc, :], T, bkeys("HT", 0, T))
        return finish()

    def fence(tag):
        P.fence()

    XRb = XR[:].bitcast(BF16)
    SCRb = SCR[:].bitcast(BF16)
    MIXv = MIX[:].rearrange("p (k t) -> p k t", k=KC)
    wv0 = w_in0.rearrange("(k p) c -> p k c", p=128)
    cnt.update({"st": 0, "po": 0, "pt": 0, "bt": 0, "sbt": 0, "pj": 0})

    fence("na")
    if stop == "fence":
        tap(HT[:, 0, :], T, bkeys("HT", 0, T))
        return finish()
    NAGs = SML[:, 8:10]
    ts(NAGs[:, 0:1], SML[:, 2:3], 0.125, None, ALU.mult, None, [("na_g",)], [("nags",)])
    cp(NAGs[:, 1:2], SML[:, 3:4], [("na_g",), ("nags",)], [("nags",)])
    for ub in range(2):
        VAb = XRb[:, ub * 9216 + 4608:ub * 9216 + 9216].rearrange("p (t h c) -> p t h c", t=NT, h=2)
        memset(VAb[:, :, 0, 64:128], 1.0, [("nav", ub)])
        memset(VAb[:, :, 1, 0:64], 1.0, [("nav", ub)])
    if stop == "fence2":
        tap(XRb[:, 4608:4608 + 2304], T, [("nav", 0)])
        return finish()
    PTr = SCRb[:, 0:2048].rearrange("p (s t) -> p s t", s=4)
    RC = SCR[:, 1024:1536]
    SQN = SCRb[:, 3072:3584]
    RSTD = SCR[:, 2048:2560]
    BTr = XR[:, 15360:17408].rearrange("p (s t) -> p s t", s=4)
    SBr = XR[:, 17408:18432].rearrange("p (s t) -> p s t", s=2)

    def na_views(u):
        ub = u % 2
        base = ub * 9216
        qT = XRb[:, base:base + 2304]
        kT = XRb[:, base + 2304:base + 4608]
        VA = XRb[:, base + 4608:base + 9216].rearrange("p (t h c) -> p t h c", t=NT, h=2)
        W = XRb[:, 18432 + ub * 3072:18432 + (ub + 1) * 3072].rearrange("p (k c) -> p k c", k=KC)
        return ub, qT, kT, VA, W

    def na_proj_stages(u):
        ub, qT, kT, VA, W = na_views(u)
        WS = XR[:, 12288:15360].rearrange("p (k c) -> p k c", k=KC)
        st = []

        def s_load():
            for i, c0 in enumerate((u * 128, 512 + u * 128, 1024 + u * 128)):
                dma(WS[:, :, i * 128:(i + 1) * 128], wv0[:, :, c0:c0 + 128], [], [("naws", i)])
                cp(W[:, :, i * 128:(i + 1) * 128], WS[:, :, i * 128:(i + 1) * 128], [("naws", i)], [("naw", ub, i)], eng="pool")
        st.append(s_load)
        st.append(lambda: None)
        st.append(lambda: None)
        for which, dst, dkey in ((0, qT, "naq"), (1, kT, "nak")):
            for i in range(5):
                t0, n = TT[i]
                pi = cnt["pj"] % 2
                cnt["pj"] += 1

                def s0(which=which, t0=t0, n=n, pi=pi):
                    for kc in range(KC):
                        mm(PS[pi][:, 0:n], W[:, kc, which * 128:(which + 1) * 128], HT[:, kc, t0:t0 + n], kc == 0, kc == KC - 1,
                           [("naw", ub, which)] + bkeys("HT", t0, n), [("ps", pi)])

                def s1(n=n, pi=pi):
                    act(SQN[:, 0:n], PS[pi][:, 0:n], AF.Square, [("ps", pi)], [("sqn",)])

                def s2(n=n):
                    mm(PS[2][:, 0:n], bd64b, SQN[:, 0:n], True, True, [("sqn",), ("CSTB",)], [("ps", 2)])

                def s3(n=n):
                    act(RSTD[:, 0:n], PS[2][:, 0:n], AF.Ln, [("ps", 2)], [("rstd",)], scale=1.0 / 64, bias=EPS)
                    act(RSTD[:, 0:n], RSTD[:, 0:n], AF.Exp, [("rstd",)], [("rstd",)], scale=-0.5)

                def s4(which=which, dst=dst, dkey=dkey, t0=t0, n=n, pi=pi, i=i):
                    stt(dst[:, t0:t0 + n], PS[pi][:, 0:n], NAGs[:, which:which + 1], RSTD[:, 0:n], ALU.mult, ALU.mult,
                        [("ps", pi), ("rstd",), ("nags",)], [(dkey, ub, i)])
                st += [s0, s1, s2, s3, s4]
        for g in range(5):
            nb = 4 if g < 4 else 2
            pi = cnt["pj"] % 2
            cnt["pj"] += 1

            def v0(g=g, nb=nb, pi=pi):
                for b4 in range(nb):
                    tb = g * 4 + b4
                    for kc in range(KC):
                        mm(PS[pi][:, b4 * 128:(b4 + 1) * 128], HT[:, kc, tb * 128:(tb + 1) * 128], W[:, kc, 256:384], kc == 0, kc == KC - 1,
                           [("naw", ub, 2), ("HT", tb)], [("ps", pi)])

            def v1(g=g, nb=nb, pi=pi):
                pv = PS[pi][:, 0:nb * 128].rearrange("p (b c) -> p b c", c=128)
                cp(VA[:, g * 4:g * 4 + nb, 0, 0:64], pv[:, :, 0:64], [("ps", pi)], [("nav", ub)], eng="act")
                cp(VA[:, g * 4:g * 4 + nb, 1, 64:128], pv[:, :, 64:128], [("ps", pi)], [("nav", ub)], eng="dve")
            st += [v0, v1]
        return st

    def na_unit(u, bg):
        ub, qT, kT, VA, W = na_views(u)
        if stop == "naproj":
            tap(qT, T, [("naq", ub, i) for i in range(5)])
            tap(kT, T, [("nak", ub, i) for i in range(5)])
            for tb in range(NT):
                tap(VA[:, tb, 0, :], 128, [("nav", ub)])
            return
        its = []
        for hh in range(2):
            for blk in range(5):
                if blk == 0:
                    q0, nq, qi = 0, 256, 0
                    keyt = [(0, None), (1, None)]
                else:
                    m = blk - 1
                    q0, nq, qi = 256 + m * 512, 512, blk
                    keyt = [(0, None), (1, None)] + [(2 + j, na_tile_index(m, j)) for j in na_key_tiles(m)]
                for ki, (kt, bi) in enumerate(keyt):
                    its.append((hh, blk, q0, nq, qi, ki, len(keyt), kt, bi))

        def st_mm(x):
            hh, blk, q0, nq, qi, ki, nk, kt, bi = its[x]
            p0 = hh * 64
            si = 3 + x % 2
            kti = 0 if kt < 2 else 1 + (kt - 2) // 4
            mm(PS[si][:, 0:nq], kT[p0:p0 + 64, kt * 128:(kt + 1) * 128], qT[p0:p0 + 64, q0:q0 + nq], True, True,
               [("nak", ub, kti), ("naq", ub, qi)], [("ps", si)])

        st_mm(0)
        nblk = 0
        for x, (hh, blk, q0, nq, qi, ki, nk, kt, bi) in enumerate(its):
            h = 2 * u + hh
            p0 = hh * 64
            dn = 64 - p0
            si = 3 + x % 2
            pst = PS[si]
            if ki == 0:
                oi = 5 + cnt["po"] % 2
                cnt["po"] += 1
            po = PS[oi]
            if x + 1 < len(its):
                st_mm(x + 1)
            pti = cnt["pt"] % 4
            cnt["pt"] += 1
            if bi is None:
                act(PTr[:, pti, 0:nq], pst[:, 0:nq], AF.Exp, [("ps", si)], [("pt", pti)])
            else:
                bti = cnt["bt"] % 4
                cnt["bt"] += 1
                dma(BTr[:, bti, :], btab[h, bi], [], [("bt", bti)])
                sbi = cnt["sbt"] % 2
                cnt["sbt"] += 1
                tt(SBr[:, sbi, :], pst[:, 0:nq], BTr[:, bti, :], ALU.add, [("ps", si), ("bt", bti)], [("sbt", sbi)])
                act(PTr[:, pti, 0:nq], SBr[:, sbi, :], AF.Exp, [("sbt", sbi)], [("pt", pti)])
            mm(po[:, 0:nq], VA[:, kt, hh, :], PTr[:, pti, 0:nq], ki == 0, ki == nk - 1,
               [("nav", ub), ("pt", pti)], [("ps", oi)])
            if x % 3 == 1:
                ada_bg()
            if bg:
                bg.pop(0)()
            if ki == nk - 1:
                act(RC[dn:dn + 64, 0:nq], po[dn:dn + 64, 0:nq], AF.Ln, [("ps", oi)], [("rc",)])
                act(RC[dn:dn + 64, 0:nq], RC[dn:dn + 64, 0:nq], AF.Exp, [("rc",)], [("rc",)], scale=-1.0)
                tt(MIXv[p0:p0 + 64, u, q0:q0 + nq], po[p0:p0 + 64, 0:nq], RC[dn:dn + 64, 0:nq], ALU.mult,
                   [("ps", oi), ("rc",)], [("MIX", u, blk)])

    for f in na_proj_stages(0):
        f()
    for u in range(4):
        bg = na_proj_stages(u + 1) if u + 1 < 4 else []
        na_unit(u, bg)
        while bg:
            bg.pop(0)()
        if stop == "naproj":
            return finish()
    ada_finish()
    if stop == "na":
        for u in range(4):
            tap(MIXv[:, u, :], T, [("MIX", u, b) for b in range(5)])
        return finish()

    fence("ml")
    T1 = XR[:, 0:2304]
    T2 = XR[:, 2304:4608]
    T3 = XR[:, 4608:6912]
    T4 = XR[:, 6912:9216]
    WGs = XR[:, 9216:10240].rearrange("p (k c) -> p k c", k=KC)
    WGb = XRb[:, 20480:21504].rearrange("p (k c) -> p k c", k=KC)
    KWT = SCR[:, 4608:5040].rearrange("p (t q r) -> p t q r", t=NT, q=3)
    MC = SCR[:, 5040:5058]
    MN = SCR[:, 5058:5076]
    SCN = SCR[:, 5076:5094]
    NEGB = SCR[:, 5094:5095]
    ZZ = SCR[:, 5096:5240].rearrange("p (r c) -> p r c", r=8)
    SCB = SCR[:, 5240:5384]
    selr = CST[:, 896:904]
    dma(WGs, wgate.rearrange("(k p) c -> p k c", p=128), [], [("wgs",)])
    cp(WGb, WGs, [("wgs",)], [("wgb",)], eng="pool")
    ts(NEGB[0:64, :], SML[0:64, 1:2], -1.0, None, ALU.mult, None, [("gateb",)], [("negb",)])
    memset(T2[0:64, :], 0.0, [("T2",)], eng="pool")
    memset(MC[0:64, :], 0.0, [("MC",)], eng="pool")
    memset(MN[0:64, :], 0.0, [("MN",)], eng="pool")
    for i in range(5):
        t0, n = TT[i]
        for kc in range(KC):
            mm(PS[0][0:64, 0:n], WGb[:, kc, 0:64], HT[:, kc, t0:t0 + n], kc == 0, kc == KC - 1, [("wgb",)] + bkeys("HT", t0, n), [("ps", 0)])
        for kc in range(KC):
            mm(PS[1][0:64, 0:n], WGb[:, kc, 64:128], HT[:, kc, t0:t0 + n], kc == 0, kc == KC - 1, [("wgb",)] + bkeys("HT", t0, n), [("ps", 1)])
        ts(T3[0:64, t0:t0 + n], PS[0][0:64, 0:n], SML[0:64, 0:1], None, ALU.add, None, [("ps", 0), ("gateb",)], [("T3",)])
        act(T1[0:64, t0:t0 + n], PS[1][0:64, 0:n], AF.Exp, [("ps", 1), ("negb",)], [("T1",)], scale=-1.0, bias=NEGB[0:64, 0:1])
    act(T1[0:64, :], T1[0:64, :], AF.Ln, [("T1",)], [("T1",)], bias=1.0)

    def scan(out, d0, d1, init, op0, op1, reads, writes):
        return P.add("dve", lambda e: e.tensor_tensor_scan(out=out, data0=d0, data1=d1, initial=init, op0=op0, op1=op1), reads, writes)

    def ones_b(p0, n):
        return ones_f[p0:p0 + 4, 0:1].to_broadcast([4, n])

    scan(T2[0:4, :], ones_b(0, T), T1[0:4, :], 0.0, ALU.mult, ALU.add, [("T1",), ("CST",), ("T2",)], [("T2",)])
    scan(T2[32:36, 0:256][:, ::-1], ones_b(32, 256), T1[32:36, 0:256][:, ::-1], 0.0, ALU.mult, ALU.add, [("T1",), ("CST",), ("T2",)], [("T2",)])
    scan(T2[32:36, 256:T][:, ::-1], ones_b(32, S_LAT), T1[32:36, 256:T][:, ::-1], T2[32:36, 0:1], ALU.mult, ALU.add,
         [("T1",), ("CST",), ("T2",)], [("T2",)])
    tt(T3[0:64, :], T3[0:64, :], T2[0:64, :], ALU.add, [("T3",), ("T2",)], [("T3",)])
    scan(T4[0:4, :], T3[0:4, :], T3[0:4, :], -1e30, ALU.max, ALU.max, [("T3",)], [("T4",)])
    scan(T4[32:36, 0:256][:, ::-1], T3[32:36, 0:256][:, ::-1], T3[32:36, 0:256][:, ::-1], -1e30, ALU.max, ALU.max, [("T3",), ("T4",)], [("T4",)])
    scan(T4[32:36, 256:T][:, ::-1], T3[32:36, 256:T][:, ::-1], T3[32:36, 256:T][:, ::-1], T4[32:36, 0:1], ALU.max, ALU.max,
         [("T3",), ("T4",)], [("T4",)])
    U3 = lambda tl, a, b: tl[a:b, :].rearrange("p (c j) -> p c j", j=128)
    cp(MC[0:4, :], U3(T4, 0, 4)[:, :, 127], [("T4",), ("MC",)], [("MC",)])
    cp(MC[32:36, :], U3(T4, 32, 36)[:, :, 0], [("T4",), ("MC",)], [("MC",)])
    cp(MN[0:4, 0:17], MC[0:4, 1:18], [("MC",), ("MN",)], [("MN",)])
    cp(MN[0:4, 17:18], MC[0:4, 17:18], [("MC",), ("MN",)], [("MN",)])
    cp(MN[32:36, 1:2], MC[32:36, 0:1], [("MC",), ("MN",)], [("MN",)])
    cp(MN[32:36, 0:1], MC[32:36, 17:18], [("MC",), ("MN",)], [("MN",)])
    cp(MN[32:36, 3:18], MC[32:36, 2:17], [("MC",), ("MN",)], [("MN",)])
    cp(MN[32:36, 2:3], MC[32:36, 2:3], [("MC",), ("MN",)], [("MN",)])
    bc = lambda v: v[0:64, :].unsqueeze(2).to_broadcast([64, NT, 128])
    tt(U3(T1, 0, 64), U3(T3, 0, 64), bc(MC), ALU.subtract, [("T3",), ("MC",), ("T1",)], [("T1",)])
    act(T1[0:64, :], T1[0:64, :], AF.Exp, [("T1",)], [("T1",)])
    tt(U3(T3, 0, 64), U3(T3, 0, 64), bc(MN), ALU.subtract, [("T3",), ("MN",)], [("T3",)])
    act(T3[0:64, :], T3[0:64, :], AF.Exp, [("T3",)], [("T3",)])
    tt(U3(T2, 0, 64), U3(T2, 0, 64), bc(MC), ALU.subtract, [("T2",), ("MC",)], [("T2",)])
    act(T2[0:64, :], T2[0:64, :], AF.Exp, [("T2",)], [("T2",)])
    tt(SCN[0:64, :], MC[0:64, :], MN[0:64, :], ALU.subtract, [("MC",), ("MN",)], [("SCN",)])
    act(SCN[0:64, :], SCN[0:64, :], AF.Exp, [("SCN",)], [("SCN",)])
    tt(ZZ[0:64], SCN[0:64, :].unsqueeze(1).to_broadcast([64, 8, NT]), selr[0:64, :].unsqueeze(2).to_broadcast([64, 8, NT]), ALU.mult,
       [("SCN",), ("CST",)], [("ZZ",)])
    mm(PS[2][:, 0:144], ones_f[0:64, :], ZZ[0:64].rearrange("p r c -> p (r c)"), True, True, [("ZZ",), ("CST",)], [("ps", 2)])
    cp(SCB, PS[2][:, 0:144], [("ps", 2)], [("SCB",)])
    for tb in range(NT):
        pi = 3 + tb % 2
        for qi, Tq, key in ((0, T1, "T1"), (1, T3, "T3"), (2, T2, "T2")):
            tr(PS[pi][:, qi * 64:(qi + 1) * 64], Tq[0:64, tb * 128:(tb + 1) * 128], ident[0:64, 0:64], [(key,), ("CST",)], [("ps", pi)])
        pv = PS[pi][:, 0:192].rearrange("p (q r) -> p q r", r=64)
        cp(KWT[:, tb, :, 0:4], pv[:, :, 0:4], [("ps", pi)], [("KWT", tb)])
        cp(KWT[:, tb, :, 4:8], pv[:, :, 32:36], [("ps", pi)], [("KWT", tb)], eng="act")
    if stop == "mlscan":
        tap(KWT.rearrange("p t q r -> p (t q r)"), 432, [("KWT", tb) for tb in range(NT)])
        tap(SCB, 144, [("SCB",)])
        return finish()

    fence("mlu")
    mqT = XRb[:, 0:2304]
    mkT = XRb[:, 2304:4608]
    KT2 = XRb[:, 4608:9216].rearrange("p (t d c) -> p t d c", t=NT, d=2)
    MVA = XRb[:, 9216:13896].rearrange("p (t h c) -> p t h c", t=NT, h=2)
    MW = XRb[:, 13896:20040].rearrange("p (k c) -> p k c", k=KC)
    MWS = XR[:, 10240:12288].rearrange("p (k c) -> p k c", k=KC)
    HF = XR[:, 12288:16896].rearrange("p (t c) -> p t c", t=NT)
    SIG = SCRb[:, 0:4608].rearrange("p (t c) -> p t c", t=NT)
    PTm2 = SCRb[:, 4608:5120].rearrange("p (s h c) -> p s h c", s=2, h=2)
    XS = SCR[:, 5400:5658].rearrange("p (d c) -> p d c", d=2)
    XB = SCRb[:, 11400:11658].rearrange("p (d c) -> p d c", d=2)
    DD = SCR[:, 5900:5902]
    DN = SCR[:, 5904:5906]
    HS = SCR[:, 6000:6256]
    HQ = SCR[:, 6256:6512]
    SS = SCR[:, 6512:6514]
    HMb = SCRb[:, 14400:14656]
    MLG = SCR[:, 6656:7168]
    dma(MLG, ml_g[:, :], [], [("MLG",)])
    memset(MVA[:, :, :, 128:130], 1.0, [("mva",)])

    def ml_unit(u):
        pend_tr = []
        try:
            ml_unit_body(u, pend_tr)
        finally:
            pass
        while pend_tr:
            pend_tr.pop(0)()

    def ml_unit_body(u, pend_tr):
        pieces = ((0, 0, 1536 + u * 128, 128), (0, 128, 1792 + u * 128, 128), (1, 256, 2048 + u * 256, 256), (2, 512, 2560 + u * 256, 256))
        for pi_, (pc, wc0, c0, ncol) in enumerate(pieces):
            dma(MWS[:, :, 0:ncol], wv0[:, :, c0:c0 + ncol], [], [("mws",)])
            cp(MW[:, :, wc0:wc0 + ncol], MWS[:, :, 0:ncol], [("mws",)], [("mw", wc0)], eng="pool")
        for which, dst, dkey, scl in ((0, mqT, "mq", 0.125), (1, mkT, "mk", 1.0)):
            for i in range(5):
                t0, n = TT[i]
                pi = cnt["pj"] % 2
                cnt["pj"] += 1
                for kc in range(KC):
                    mm(PS[pi][:, 0:n], MW[:, kc, which * 128:(which + 1) * 128], HT[:, kc, t0:t0 + n], kc == 0, kc == KC - 1,
                       [("mw", which * 128)] + bkeys("HT", t0, n), [("ps", pi)])
                act(dst[:, t0:t0 + n], PS[pi][:, 0:n], AF.Copy, [("ps", pi)], [(dkey, i)], scale=scl)
        if stop == "mlA":
            tap(mqT, T, [("mq", i) for i in range(5)])
            return
        for tb in range(NT):
            pi = cnt["pj"] % 2
            cnt["pj"] += 1
            ps = PS[pi]
            for kc in range(KC):
                mm(ps[:, 0:128], HT[:, kc, tb * 128:(tb + 1) * 128], MW[:, kc, 128:256], kc == 0, kc == KC - 1, [("mw", 128), ("HT", tb)], [("ps", pi)])
            for kc in range(KC):
                mm(ps[:, 128:384], HT[:, kc, tb * 128:(tb + 1) * 128], MW[:, kc, 256:512], kc == 0, kc == KC - 1, [("mw", 256), ("HT", tb)], [("ps", pi)])
            for dr in range(2):
                if os.environ.get("SKIP") == "kt2":
                    break
                tt(KT2[:, tb, dr, :].rearrange("p (h c) -> p h c", h=2), ps[:, 0:128].rearrange("p (h c) -> p h c", h=2),
                   KWT[:, tb, 1, dr * 4 + 2 * u:dr * 4 + 2 * u + 2].unsqueeze(2).to_broadcast([128, 2, 64]), ALU.mult,
                   [("ps", pi), ("KWT", tb)], [("kt2", tb)])
            if os.environ.get("SKIP") != "mva":
                cp(MVA[:, tb, :, 0:128], ps[:, 128:384].rearrange("p (h c) -> p h c", h=2), [("ps", pi)], [("mva",)], eng="act")
            pi = cnt["pj"] % 2
            cnt["pj"] += 1
            ps = PS[pi]
            for kc in range(KC):
                mm(ps[:, 0:256], HT[:, kc, tb * 128:(tb + 1) * 128], MW[:, kc, 512:768], kc == 0, kc == KC - 1, [("mw", 512), ("HT", tb)], [("ps", pi)])
            if os.environ.get("SKIP") != "sig":
                act(SIG[:, tb, :], ps[:, 0:256], AF.Sigmoid, [("ps", pi)], [("sig", tb)])
        if stop == "mlB":
            tap(SIG[:, 0, :], 256, [("sig", 0)])
            tap(KT2[:, 0, 1, :], 128, [("kt2", 0)])
            tap(MVA[:, 3, 1, 0:129], 129, [("mva",)])
            return
        for dr in range(2):
            order = list(range(NT)) if dr == 0 else [1, 0] + list(range(17, 1, -1))
            if stop == "mlC":
                if dr == 1:
                    break
                order = order[:2]
            tri = trif if dr == 0 else trib
            def st_a(sx):
                cx = order[sx]
                cix = 0 if cx < 2 else 1 + (cx - 2) // 4
                cslx = slice(cx * 128, (cx + 1) * 128)
                for hh in range(2):
                    p0 = hh * 64
                    sbk = 2 if hh == 0 else 6
                    mm(PS[sbk][:, 0:128], mkT[p0:p0 + 64, cslx], mqT[p0:p0 + 64, cslx], True, True,
                       [("mk", cix), ("mq", cix)], [("ps", sbk)])
                for hh in range(2):
                    r = dr * 4 + 2 * u + hh
                    sbk = 2 if hh == 0 else 6
                    stt(PTm2[:, sx % 2, hh, :], PS[sbk][:, 0:128], KWT[:, cx, 0, r:r + 1], tri, ALU.mult, ALU.mult,
                        [("ps", sbk), ("KWT", cx), ("CST",)], [("ptm", sx % 2, hh)])

            st_a(0)
            for s_, c in enumerate(order):
                ci = 0 if c < 2 else 1 + (c - 2) // 4
                csl = slice(c * 128, (c + 1) * 128)
                if s_ + 1 < len(order):
                    st_a(s_ + 1)
                OB = (4, 5) if s_ % 2 == 0 else (0, 1)
                for hh in range(2):
                    p0 = hh * 64
                    mm(PS[3][p0:p0 + 64, 0:129], KT2[:, c, dr, hh * 64:(hh + 1) * 64], MVA[:, c, hh, 0:129], True, True,
                       [("kt2", c), ("mva",)], [("ps", 3)])
                for hh in range(2):
                    p0 = hh * 64
                    mm(PS[OB[hh]][:, 0:129], PTm2[:, s_ % 2, hh, :], MVA[:, c, hh, 0:129], True, s_ == 0,
                       [("ptm", s_ % 2, hh), ("mva",)], [("ps", OB[hh])])
                    if s_ > 0:
                        mm(PS[OB[hh]][:, 0:129], mqT[p0:p0 + 64, csl], XB[p0:p0 + 64, dr, :], False, True,
                           [("mq", ci), ("xb", hh)], [("ps", OB[hh])])
                while pend_tr:
                    pend_tr.pop(0)()
                for hh in range(2):
                    p0 = hh * 64
                    r = dr * 4 + 2 * u + hh
                    if s_ == 0:
                        cp(XS[p0:p0 + 64, dr, :], PS[3][p0:p0 + 64, 0:129], [("ps", 3)], [("xs", hh)])
                    else:
                        stt(XS[p0:p0 + 64, dr, :], XS[p0:p0 + 64, dr, :], SCB[p0:p0 + 64, r * NT + c:r * NT + c + 1], PS[3][p0:p0 + 64, 0:129],
                            ALU.mult, ALU.add, [("xs", hh), ("SCB",), ("ps", 3)], [("xs", hh)])
                    cp(XB[p0:p0 + 64, dr, :], XS[p0:p0 + 64, dr, :], [("xs", hh)], [("xb", hh)], eng="pool")
                for hh in range(2):
                    cp(DN[:, hh:hh + 1], PS[OB[hh]][:, 128:129], [("ps", OB[hh])], [("dn",)])
                r0 = dr * 4 + 2 * u
                stt(DD, DN, -1.0, DN, ALU.mult, ALU.max, [("dn",)], [("dd",)])
                tt(DD, DD, KWT[:, c, 2, r0:r0 + 2], ALU.max, [("dd",), ("KWT", c)], [("dd",)])
                recip(DD, DD, [("dd",)], [("dd",)])
                for hh in range(2):
                    hs = slice(hh * 128, (hh + 1) * 128)
                    if dr == 0:
                        act(HF[:, c, hs], PS[OB[hh]][:, 0:128], AF.Copy, [("ps", OB[hh]), ("dd",)], [("hf", c)], scale=DD[:, hh:hh + 1])
                    else:
                        stt(HS[:, hs], PS[OB[hh]][:, 0:128], DD[:, hh:hh + 1], HF[:, c, hs], ALU.mult, ALU.add,
                            [("ps", OB[hh]), ("dd",), ("hf", c)], [("hs",)])
                if dr == 1:
                    for hh in range(2):
                        P.add("act", lambda e, hh=hh: e.activation(out=HQ[:, hh * 128:(hh + 1) * 128], in_=HS[:, hh * 128:(hh + 1) * 128],
                                                                   func=AF.Square, accum_out=SS[:, hh:hh + 1]),
                              [("hs",), ("hq",), ("ss",)], [("hq",), ("ss",)])
                    act(SS, SS, AF.Ln, [("ss",)], [("ss",)], scale=1.0 / 128, bias=EPS)
                    act(SS, SS, AF.Exp, [("ss",)], [("ss",)], scale=-0.5)
                    for hh in range(2):
                        act(HS[:, hh * 128:(hh + 1) * 128], HS[:, hh * 128:(hh + 1) * 128], AF.Copy, [("hs",), ("ss",)], [("hs",)],
                            scale=SS[:, hh:hh + 1])
                    tt(HS, HS, MLG[:, u * 256:(u + 1) * 256], ALU.mult, [("hs",), ("MLG",)], [("hs",)])
                    tt(HMb, HS, SIG[:, c, :], ALU.mult, [("hs",), ("sig", c)], [("hmb",)])
                    def tr_out(c=c, csl=csl):
                        for hh in range(2):
                            tr(PSB[:, hh * 128:(hh + 1) * 128], HMb[:, hh * 128:(hh + 1) * 128], identb, [("hmb",), ("CSTB",)], [("psb",)])
                        for hh in range(2):
                            cp(MIXv[:, 4 + 2 * u + hh, csl], PSB[:, hh * 128:(hh + 1) * 128], [("psb",)], [("MIXm", 2 * u + hh, c)], eng="act")
                    pend_tr.append(tr_out)

    for u in range(2):
        ml_unit(u)
        if stop in ("ml1", "mlA", "mlB", "mlC"):
            break
    if stop == "mlC":
        tap(HF[:, 0, :], 256, [("hf", 0)])
        tap(HF[:, 1, :], 256, [("hf", 1)])
        return finish()
    if stop in ("mlA", "mlB"):
        return finish()
    if stop in ("ml", "ml1"):
        for j in range(4, 8):
            tap(MIXv[:, j, :], T, [("MIXm", j - 4, c) for c in range(NT)])
        return finish()

    def out_proj(l, w_out, xold_view, tiles, mix_keys, xold_keys=lambda j: []):
        fence("wo%d" % l)
        WO = SCRb[:, 0:8192].rearrange("p (k c) -> p k c", k=KC)
        WOS = SCR[:, 4096:6144].rearrange("p (k c) -> p k c", k=KC)
        XO = SCR[:, 6144:8192].rearrange("p (s t) -> p s t", s=4)
        wv = w_out.rearrange("(k p) c -> p k c", p=128)
        for pc in range(4):
            dma(WOS, wv[:, :, pc * 256:(pc + 1) * 256], [], [("wos",)])
            cp(WO[:, :, pc * 256:(pc + 1) * 256], WOS, [("wos",)], [("wo", pc)], eng="pool")
        for i in tiles:
            t0, n = TT[i]
            seg = 1 if i == 0 else 0
            for j in range(KC):
                pi = cnt["pj"] % 2
                cnt["pj"] += 1
                for kc in range(KC):
                    mm(PS[pi][:, 0:n], WO[:, kc, j * 128:(j + 1) * 128], MIXv[:, kc, t0:t0 + n], kc == 0, kc == KC - 1,
                       [("wo", j // 2)] + mix_keys(i), [("ps", pi)])
                xi = cnt["bt"] % 4
                cnt["bt"] += 1
                dma(XO[:, xi, 0:n], xold_view[:, j, t0:t0 + n], xold_keys(j), [("xo", xi)])
                stt(X[:, j, t0:t0 + n], PS[pi][:, 0:n], MOD[:, l, 16 + j, seg:seg + 1], XO[:, xi, 0:n], ALU.mult, ALU.add,
                    [("ps", pi), ("MOD", l), ("xo", xi)], [("X", i, j)])

    def mix_keys0(i):
        t0, n = TT[i]
        return [("MIX", u, i) for u in range(4)] + [("MIXm", jj, c) for jj in range(4) for c in range(t0 // 128, (t0 + n) // 128)]

    out_proj(0, w_out0, xv, range(5), mix_keys0)
    if stop == "wo0":
        for j in range(KC):
            tap(X[:, j, :], T, [("X", i, j) for i in range(5)])
        return finish()

    RW = sb("RW", [128, KC, NE], F32)
    dma(RW[:], router_w.rearrange("(k p) e -> p k e", p=128), [], [("RW",)])
    COMB = sb("COMB", [128, NT, NE], F32)
    WX = sb("WX", [128, 6144], BF16)

    def moe(l, tiles):
        fence("moe%d" % l)
        nt0 = TT[tiles[0]][0] // 128
        ntb = NT - nt0
        H32 = MIX[:, 0:8192].bitcast(F32).rearrange("p (k t) -> p k t", k=KC)
        LGS = SML[:, 64:64 + 0]

        def h32_cb(i, sq, s2, t0, n):
            seg = 1 if i == 0 else 0
            for kc in range(KC):
                ts(H32[:, kc, 0:n], sq[:, kc, :], GS[:, l, 1, kc, seg:seg + 1], MOD[:, l, 24 + kc, seg:seg + 1], ALU.mult, ALU.add,
                   [("sq", s2), ("GS", l, 1), ("MOD", l)], [("h32",)])
            for b in range(n // 128):
                tb = t0 // 128 + b
                for kc in range(KC):
                    mm(PS[6][:, tb * NE:(tb + 1) * NE], H32[:, kc, b * 128:(b + 1) * 128], RW[:, kc, :], kc == 0, kc == KC - 1,
                       [("h32",), ("RW",)], [("ps", 6)])

        def src_x(i):
            t0, n = TT[i]
            return X[:, :, t0:t0 + n], [("X", i, j) for j in range(KC)]

        norm_modulate(l, 1, src_x, tiles, h32_cb)
        RT = SCR[:, 0:4608]
        SC_ = RT[:, 0:288].rearrange("p (t e) -> p t e", e=NE)
        SEL = RT[:, 288:576].rearrange("p (t e) -> p t e", e=NE)
        EQ = RT[:, 576:864].rearrange("p (t e) -> p t e", e=NE)
        SEL2 = RT[:, 864:1152].rearrange("p (t e) -> p t e", e=NE)
        M1 = RT[:, 1152:1224]
        M2 = RT[:, 1224:1296]
        GSC = RT[:, 1296:1368]
        GMX = RT[:, 1368:1386]
        GMK = RT[:, 1386:1458]
        WS_ = RT[:, 1458:1476]
        g4 = lambda a: a.rearrange("p t (g j) -> p (t g) j", j=4)
        act(SC_, PS[6][:, 0:288].rearrange("p (t e) -> p t e", e=NE), AF.Sigmoid, [("ps", 6)], [("r_sc",)])
        tt(SEL, SC_, SML[:, 16:32].unsqueeze(1).to_broadcast([128, NT, NE]), ALU.add, [("r_sc",), ("router_b",)], [("r_sel",)])
        red(M1, g4(SEL), ALU.max, [("r_sel",)], [("r_m1",)])
        tt(g4(EQ), g4(SEL), M1.unsqueeze(2).to_broadcast([128, 72, 4]), ALU.is_equal, [("r_sel",), ("r_m1",)], [("r_eq",)])
        stt(SEL2, EQ, -1e9, SEL, ALU.mult, ALU.add, [("r_eq",), ("r_sel",)], [("r_sel2",)])
        red(M2, g4(SEL2), ALU.max, [("r_sel2",)], [("r_m2",)])
        tt(GSC, M1, M2, ALU.add, [("r_m1",), ("r_m2",)], [("r_gsc",)])
        red(GMX, GSC.rearrange("p (t g) -> p t g", g=4), ALU.max, [("r_gsc",)], [("r_gmx",)])
        tt(GMK.rearrange("p (t g) -> p t g", g=4), GSC.rearrange("p (t g) -> p t g", g=4), GMX.unsqueeze(2).to_broadcast([128, NT, 4]),
           ALU.is_equal, [("r_gsc",), ("r_gmx",)], [("r_gmk",)])
        tt(g4(EQ), g4(SEL), M2.unsqueeze(2).to_broadcast([128, 72, 4]), ALU.is_ge, [("r_sel",), ("r_m2",), ("r_eq",)], [("r_eq",)])
        tt(g4(EQ), g4(EQ), GMK.unsqueeze(2).to_broadcast([128, 72, 4]), ALU.mult, [("r_eq",), ("r_gmk",)], [("r_eq",)])
        tt(SEL2, SC_, EQ, ALU.mult, [("r_sc",), ("r_eq",), ("r_sel2",)], [("r_sel2",)])
        red(WS_, SEL2, ALU.add, [("r_sel2",)], [("r_ws",)])
        recip(WS_, WS_, [("r_ws",)], [("r_ws",)])
        tt(COMB[:], SEL2, WS_.unsqueeze(2).to_broadcast([128, NT, NE]), ALU.mult, [("r_sel2",), ("r_ws",)], [("COMB",)])
        if stop == "route%d" % l:
            tap(COMB[:].rearrange("p t e -> p (t e)"), 288, [("COMB",)])
            return True
        fence("moe%de" % l)
        MIXb = MIX[:]
        HSL = []
        for sl in range(4):
            HSL.append((MIXb[:, sl * 4096:sl * 4096 + 2048], MIXb[:, sl * 4096 + 2048:(sl + 1) * 4096]))
        HSL.append((WX[:, 0:2048], WX[:, 2048:4096]))
        HSL.append((WX[:, 4096:6144], MIXb[:, 16384:18432]))
        STG = SCR[:, 0:4096].rearrange("p (s c) -> p s c", s=2)
        GB = SCRb[:, 8192:12288].rearrange("p (s f t) -> p s f t", s=2, f=4)
        AR = SCRb[:, 12288:13312].rearrange("p (s t) -> p s t", s=2)
        TM = SCRb[:, 13312:14336].rearrange("p (s t) -> p s t", s=2)
        CB = SCRb[:, 14336:15360].rearrange("p (s t) -> p s t", s=2)
        DG = SCRb[:, 15360:15872]
        mcnt = [0, 0]

        n_exp = int(os.environ.get("NEXP", NE))
        halves = []
        WTAB = []
        for e in range(n_exp):
            mats = []
            for mi, (dram2d, kparts, ncols) in enumerate(((exp_w1[l, e], 8, DFF), (exp_w3[l, e], 8, DFF), (exp_w2[l, e], 4, D))):
                sl = (3 * e + mi) % 6
                src = dram2d.rearrange("(k p) c -> p k c", p=128)
                half = kparts // 2
                out = []
                for hf in range(2):
                    dst = HSL[sl][hf].rearrange("p (k c) -> p k c", k=half)
                    key = ("wsl", sl, hf)
                    halves.append((src[:, hf * half:(hf + 1) * half, :], dst, key, half))
                    out.append((dst, key))
                mats.append(out)
            WTAB.append(mats)
        ptr = [0, 0]

        def emit_dma():
            q = ptr[0]
            if q >= len(halves):
                return
            ptr[0] += 1
            src, dst, key, half = halves[q]
            st = STG[:, q % 2, :].rearrange("p (k c) -> p k c", k=half)
            dma(st, src, [], [("stg", q % 2)])

        def advance():
            q = ptr[1]
            if q >= len(halves):
                return
            ptr[1] += 1
            src, dst, key, half = halves[q]
            st = STG[:, q % 2, :].rearrange("p (k c) -> p k c", k=half)
            cp(dst, st, [("stg", q % 2)], [key], eng="act")
            emit_dma()

        emit_dma()
        emit_dma()
        for _ in range(6):
            advance()

        DG2 = SCRb[:, 15360:16384].rearrange("p (s t) -> p s t", s=2)

        def prep_dg(k):
            if k >= len(seq):
                return
            e, i = seq[k]
            t0, n = TT[i]
            tb0, nb = t0 // 128, n // 128
            tt(DG2[:, k % 2, 0:n].rearrange("p (b t) -> p b t", t=128), identb.unsqueeze(1).to_broadcast([128, nb, 128]),
               COMB[:, tb0:tb0 + nb, e:e + 1].to_broadcast([128, nb, 128]), ALU.mult, [("CSTB",), ("COMB",)], [("dg", k % 2)])

        def prep_cb(k):
            if k >= len(seq):
                return
            e, i = seq[k]
            t0, n = TT[i]
            mm(PS[6][:, 0:n], onesb, DG2[:, k % 2, 0:n], True, True, [("dg", k % 2), ("CSTB",)], [("ps", 6)])
            cp(CB[:, k % 2, 0:n], PS[6][:, 0:n], [("ps", 6)], [("cb", k % 2)], eng="act")

        def h_phase(k, W1, W3):
            e, i = seq[k]
            t0, n = TT[i]
            ci = k % 2
            gi = cnt["po"] % 2
            cnt["po"] += 1
            for fc in range(4):
                pa = cnt["pj"] % 2
                cnt["pj"] += 1
                pb = 2 + pa
                for kc in range(KC):
                    w, wk = W1[kc // 4]
                    mm(PS[pa][:, 0:n], w[:, kc % 4, fc * 128:(fc + 1) * 128], HT[:, kc, t0:t0 + n], kc == 0, kc == KC - 1,
                       [wk] + bkeys("HT", t0, n), [("ps", pa)])
                for kc in range(KC):
                    w, wk = W3[kc // 4]
                    mm(PS[pb][:, 0:n], w[:, kc % 4, fc * 128:(fc + 1) * 128], HT[:, kc, t0:t0 + n], kc == 0, kc == KC - 1,
                       [wk] + bkeys("HT", t0, n), [("ps", pb)])
                if fc == 2:
                    prep_cb(k + 1)
                ai = cnt["pt"] % 2
                cnt["pt"] += 1
                act(AR[:, ai, 0:n], PS[pa][:, 0:n], AF.Silu, [("ps", pa)], [("ar", ai)])
                tt(TM[:, ai, 0:n], PS[pb][:, 0:n], CB[:, ci, 0:n], ALU.mult, [("ps", pb), ("cb", ci)], [("tm", ai)])
                tt(GB[:, gi, fc, 0:n], AR[:, ai, 0:n], TM[:, ai, 0:n], ALU.mult, [("ar", ai), ("tm", ai)], [("gb", gi, fc)], eng="pool")
            return gi

        def y_phase(i, gi, W2):
            t0, n = TT[i]
            seg = 1 if i == 0 else 0
            for j in range(KC):
                py = 4 + cnt["st"] % 2
                cnt["st"] += 1
                for fc in range(4):
                    w, wk = W2[fc // 2]
                    mm(PS[py][:, 0:n], w[:, fc % 2, j * 128:(j + 1) * 128], GB[:, gi, fc, 0:n], fc == 0, fc == 3,
                       [wk, ("gb", gi, fc)], [("ps", py)])
                stt(X[:, j, t0:t0 + n], PS[py][:, 0:n], MOD[:, l, 40 + j, seg:seg + 1], X[:, j, t0:t0 + n], ALU.mult, ALU.add,
                    [("ps", py), ("MOD", l), ("X", i, j)], [("X", i, j)])

        pend = None
        tl = list(tiles)
        seq = [(e, i) for e in range(n_exp) for i in tl]
        sched = [2, 1, 1, 1, 1] if len(tl) == 5 else [2, 1, 2, 1]
        prep_dg(0)
        prep_cb(0)
        prep_dg(1)
        for k, (e, i) in enumerate(seq):
            W1, W3, W2 = WTAB[e]
            gi = h_phase(k, W1, W3)
            prep_dg(k + 2)
            if pend is not None:
                y_phase(*pend)
            pend = (i, gi, W2)
            for _ in range(sched[k % len(tl)]):
                advance()
        y_phase(*pend)
        return False

    if moe(0, range(5)):
        return finish()
    if stop == "l0":
        for j in range(KC):
            tap(X[:, j, :], T, [("X", i, j) for i in range(5)])
        return finish()

    def src_x1(i):
        t0, n = TT[i]
        return X[:, :, t0:t0 + n], [("X", i, j) for j in range(KC)]

    norm_modulate(1, 0, src_x1, range(5))
    xspv = xsp.rearrange("(k p) t -> p k t", p=128)
    for j in range(KC):
        dma(xspv[:, j, 256:T], X[:, j, 256:T], [("X", i, j) for i in range(1, 5)], [("xsp", j)])
    fence("gqa")
    wv1 = w_in1.rearrange("(k p) c -> p k c", p=128)
    gkT = XRb[:, 0:4608].rearrange("p (v t) -> p v t", v=2)
    gV = XRb[:, 4608:9216].rearrange("p (t v c) -> p t v c", t=NT, v=2)
    gQ = [XRb[:, 9216:11264], XRb[:, 11264:13312]]
    RPC = XR[:, 6656:8704]
    RPS = XR[:, 8704:10752]
    GW = XRb[:, 21504:25600].rearrange("p (s k c) -> p s k c", s=2, k=KC)
    GWS = XR[:, 12800:14848].rearrange("p (k c) -> p k c", k=KC)
    GPT = XRb[:, 29696:31744].rearrange("p (s t) -> p s t", s=4)
    QN = XRb[:, 31744:32256]
    TMPA = XR[:, 16128:16640]
    TMPB = XR[:, 16640:17152]
    GRC = XR[:, 17152:17664]
    GRS = XR[:, 17664:18176]
    GSQ = XRb[:, 36352:36864]
    GQG = SML[:, 10:12]
    dma(RPC, ropeC[:, :], [], [("rpc",)])
    dma(RPS, ropeS[:, :], [], [("rps",)])
    ts(GQG[:, 0:1], SML[:, 4:5], 128.0 ** -0.5, None, ALU.mult, None, [("gqa_g",)], [("gqg",)])
    cp(GQG[:, 1:2], SML[:, 5:6], [("gqa_g",), ("gqg",)], [("gqg",)])
    gwc = [0]

    def load_w1(c0, ncol):
        si = gwc[0] % 2
        gwc[0] += 1
        dma(GWS[:, :, 0:ncol], wv1[:, :, c0:c0 + ncol], [], [("gws",)])
        cp(GW[:, si, :, 0:ncol], GWS[:, :, 0:ncol], [("gws",)], [("gw", si)], eng="pool")
        return GW[:, si], ("gw", si)

    def qk_norm_rope(ps, pi, n, gcol, dst, rope_t0, wkeys, stat=None):
        sps, skey = (PS[6], ("ps", 6)) if stat is None else (stat[0], (stat[1],))
        act(GSQ[:, 0:n], ps[:, 0:n], AF.Square, [("ps", pi)], [("gsq",)])
        mm(sps[:, 0:n], onesb, GSQ[:, 0:n], True, True, [("gsq",), ("CSTB",)], [skey])
        act(GRS[:, 0:n], sps[:, 0:n], AF.Ln, [skey], [("grs",)], scale=1.0 / 128, bias=EPS)
        act(GRS[:, 0:n], GRS[:, 0:n], AF.Exp, [("grs",)], [("grs",)], scale=-0.5)
        if rope_t0 is None:
            stt(dst, ps[:, 0:n], GQG[:, gcol:gcol + 1], GRS[:, 0:n], ALU.mult, ALU.mult, [("ps", pi), ("grs",), ("gqg",)], wkeys)
            return
        stt(QN[:, 0:n], ps[:, 0:n], GQG[:, gcol:gcol + 1], GRS[:, 0:n], ALU.mult, ALU.mult, [("ps", pi), ("grs",), ("gqg",)], [("qn",)])
        mm(sps[:, 0:n], swapb, QN[:, 0:n], True, True, [("qn",), ("CSTB",)], [skey])
        tt(TMPA[:, 0:n], sps[:, 0:n], RPS[:, rope_t0:rope_t0 + n], ALU.mult, [skey, ("rps",)], [("tmpa",)])
        tt(TMPB[:, 0:n], QN[:, 0:n], RPC[:, rope_t0:rope_t0 + n], ALU.mult, [("qn",), ("rpc",)], [("tmpb",)])
        tt(dst, TMPA[:, 0:n], TMPB[:, 0:n], ALU.add, [("tmpa",), ("tmpb",)], wkeys)

    Wk, wkk = load_w1(1024, 256)
    for kv in range(2):
        for i in range(5):
            t0, n = TT[i]
            pi = cnt["pj"] % 2
            cnt["pj"] += 1
            for kc in range(KC):
                mm(PS[pi][:, 0:n], Wk[:, kc, kv * 128:(kv + 1) * 128], HT[:, kc, t0:t0 + n], kc == 0, kc == KC - 1,
                   [wkk] + bkeys("HT", t0, n), [("ps", pi)])
            qk_norm_rope(PS[pi], pi, n, 1, gkT[:, kv, t0:t0 + n], None if i == 0 else t0 - 256, [("gk", kv, i)])
    Wv, wvk = load_w1(1280, 256)
    for g in range(9):
        pi = cnt["pj"] % 2
        cnt["pj"] += 1
        for b2 in range(2):
            tb = g * 2 + b2
            for kc in range(KC):
                mm(PS[pi][:, b2 * 256:(b2 + 1) * 256], HT[:, kc, tb * 128:(tb + 1) * 128], Wv[:, kc, :], kc == 0, kc == KC - 1,
                   [wvk, ("HT", tb)], [("ps", pi)])
        cp(gV[:, g * 2:g * 2 + 2].rearrange("p t v c -> p t (v c)"), PS[pi][:, 0:512].rearrange("p (t c) -> p t c", t=2),
           [("ps", pi)], [("gv", g)], eng="act")
    if stop == "gqakv":
        tap(gkT[:, 0, :], T, [("gk", 0, i) for i in range(5)])
        tap(gkT[:, 1, :], T, [("gk", 1, i) for i in range(5)])
        return finish()

    PSBf = PSB[:].bitcast(F32)

    def q_stages(h):
        st = []
        qT = gQ[h % 2]
        hold = {}

        def s_load():
            hold["w"] = load_w1(h * 128, 128)
        st.append(s_load)
        for i in range(1, 5):
            t0, n = TT[i]
            r0 = t0 - 256
            dst = qT[:, r0:r0 + n]

            def s0(t0=t0, n=n):
                Wq, wqk = hold["w"]
                for kc in range(KC):
                    mm(PS[6][:, 0:n], Wq[:, kc, 0:128], HT[:, kc, t0:t0 + n], kc == 0, kc == KC - 1, [wqk] + bkeys("HT", t0, n), [("ps", 6)])

            def s1(n=n):
                act(GSQ[:, 0:n], PS[6][:, 0:n], AF.Square, [("ps", 6)], [("gsq",)])

            def s2(n=n):
                mm(PSBf[:, 0:n], onesb, GSQ[:, 0:n], True, True, [("gsq",), ("CSTB",)], [("psb",)])

            def s3(n=n):
                act(GRS[:, 0:n], PSBf[:, 0:n], AF.Ln, [("psb",)], [("grs",)], scale=1.0 / 128, bias=EPS)
                act(GRS[:, 0:n], GRS[:, 0:n], AF.Exp, [("grs",)], [("grs",)], scale=-0.5)

            def s4(n=n):
                stt(QN[:, 0:n], PS[6][:, 0:n], GQG[:, 0:1], GRS[:, 0:n], ALU.mult, ALU.mult, [("ps", 6), ("grs",), ("gqg",)], [("qn",)])

            def s5(n=n):
                mm(PSBf[:, 0:n], swapb, QN[:, 0:n], True, True, [("qn",), ("CSTB",)], [("psb",)])

            def s6(n=n, r0=r0):
                tt(TMPA[:, 0:n], PSBf[:, 0:n], RPS[:, r0:r0 + n], ALU.mult, [("psb",), ("rps",)], [("tmpa",)])
                tt(TMPB[:, 0:n], QN[:, 0:n], RPC[:, r0:r0 + n], ALU.mult, [("qn",), ("rpc",)], [("tmpb",)])

            def s7(n=n, dst=dst, i=i):
                tt(dst, TMPA[:, 0:n], TMPB[:, 0:n], ALU.add, [("tmpa",), ("tmpb",)], [("gq", h % 2, i)])

            st += [s0, s1, s2, s3, s4, s5, s6, s7]
        return st

    def q_proj(h):
        for f in q_stages(h):
            f()

    iters = [(h, m, kt) for h in range(8) for m in range(4) for kt in range(NT)]

    STB = [PS[0], PS[1], PS[5]]
    STK = [("ps", 0), ("ps", 1), ("ps", 5)]
    ACC = SCR[:, 0:2048].rearrange("p (s e t) -> p s e t", s=2, e=2)

    def emit_st(idx):
        if idx >= len(iters):
            return
        h, m, kt = iters[idx]
        si = idx % 3
        kti = 0 if kt < 2 else 1 + (kt - 2) // 4
        mm(STB[si][:, 0:512], gkT[:, h // 4, kt * 128:(kt + 1) * 128], gQ[h % 2][:, m * 512:(m + 1) * 512], True, True,
           [("gk", h // 4, kti), ("gq", h % 2, m + 1)], [STK[si]])

    q_proj(0)
    emit_st(0)
    emit_st(1)
    for idx, (h, m, kt) in enumerate(iters):
        if stop == "gqa1" and idx >= NT:
            break
        kv = h // 4
        si = idx % 3
        blk = idx // NT
        oi = 2 + blk % 2
        ai = blk % 2
        emit_st(idx + 2)
        pti = idx % 4
        act(GPT[:, pti, :], STB[si][:, 0:512], AF.Exp, [STK[si]], [("gpt", pti)])
        mm(PS[oi][:, 0:512], gV[:, kt, kv, :], GPT[:, pti, :], kt == 0, kt == NT - 1, [("gv", kt // 2), ("gpt", pti)], [("ps", oi)])
        ae = kt % 2
        aeng = "dve" if ae == 0 else "pool"
        if kt < 2:
            cp(ACC[:, ai, ae, :], GPT[:, pti, :], [("gpt", pti)], [("acc", ai, ae)], eng=aeng)
        else:
            tt(ACC[:, ai, ae, :], ACC[:, ai, ae, :], GPT[:, pti, :], ALU.add, [("acc", ai, ae), ("gpt", pti)], [("acc", ai, ae)], eng=aeng)
        if kt == NT - 1:
            mm(PS[4][:, 0:512], ones_f, ACC[:, ai, 0, :], True, False, [("CST",), ("acc", ai, 0)], [("ps", 4)])
            mm(PS[4][:, 0:512], ones_f, ACC[:, ai, 1, :], False, True, [("CST",), ("acc", ai, 1)], [("ps", 4)])
            recip(GRC, PS[4][:, 0:512], [("ps", 4)], [("grc",)])
            tt(MIXv[:, h, 256 + m * 512:256 + (m + 1) * 512], PS[oi][:, 0:512], GRC, ALU.mult, [("ps", oi), ("grc",)], [("MIXg", h, m + 1)])
        if m == 0 and kt == 0:
            pendq = q_stages(h + 1) if h + 1 < 8 else []
        if pendq and idx % 2 == 1:
            pendq.pop(0)()
    if stop == "gqa1":
        tap(MIXv[:, 0, 256:768], 512, [("MIXg", 0, 1)])
        return finish()
    if stop == "gqa":
        for h in range(8):
            tap(MIXv[:, h, 256:T], S_LAT, [("MIXg", h, m) for m in range(1, 5)])
        return finish()

    out_proj(1, w_out1, xspv, range(1, 5), lambda i: [("MIXg", h, i) for h in range(8)], lambda j: [("xsp", j)])
    if stop == "wo1":
        for j in range(KC):
            tap(X[:, j, 256:T], S_LAT, [("X", i, j) for i in range(1, 5)])
        return finish()
    if moe(1, range(1, 5)):
        return finish()
    ov = outT.rearrange("(k p) t -> p k t", p=128)
    for j in range(KC):
        dma(ov[:, j, :], X[:, j, 256:T], [("X", i, j) for i in range(1, 5)], [("out", j)])
        k_.out_keys.append(("out", j))
    return finish()


def _fm(v):
    v = np.asarray(v, np.float32)
    return np.ascontiguousarray(v.reshape(-1, 128).T)


def _const_tables():
    c = np.zeros((128, 1024), np.float32)
    c[:, 0:128] = np.eye(128, dtype=np.float32)
    c[:, 128:256] = 1.0
    j = np.arange(128)[:, None]
    i = np.arange(128)[None, :]
    c[:, 256:384] = (j <= i)
    c[:, 384:512] = (j >= i)
    sw = np.zeros((128, 128), np.float32)
    sw[(np.arange(128) + 64) % 128, np.arange(128)] = 1.0
    c[:, 512:640] = sw
    c[:, 640:768] = 1.0
    bd = np.zeros((128, 128), np.float32)
    bd[0:64, 0:64] = 1.0
    bd[64:128, 64:128] = 1.0
    c[:, 768:896] = bd
    for r in range(8):
        c[(r if r < 4 else 32 + r - 4), 896 + r] = 1.0
    return c


def _rope_tables():
    n_freq = 128 // 4
    inv_freq = (np.float32(10000.0) ** (-np.arange(n_freq, dtype=np.float32) / np.float32(n_freq))).astype(np.float32)
    t = np.arange(S_LAT)
    rows = (t // 64).astype(np.float32)
    cols = (t % 64).astype(np.float32)
    ang = np.concatenate([rows[:, None] * inv_freq, cols[:, None] * inv_freq], axis=-1).astype(np.float32)
    cos = np.cos(ang).astype(np.float32).T
    sin = np.sin(ang).astype(np.float32).T
    C = np.concatenate([cos, cos], 0)
    S = np.concatenate([-sin, sin], 0)
    return np.ascontiguousarray(C), np.ascontiguousarray(S)


def na_tile_index(m, j):
    if m == 0:
        return j
    if m == 3:
        return 14 + (j - 10)
    return 6 + (j - (4 * m - 2))


def na_key_tiles(m):
    if m == 0:
        return list(range(0, 6))
    if m == 3:
        return list(range(10, 16))
    return list(range(4 * m - 2, 4 * m + 6))


def _na_bias_tables(rpb):
    rpb = np.asarray(rpb, np.float32)
    out = np.full((8, 20, 128, 512), NEG, np.float32)
    kc = np.arange(64)
    qc = np.arange(64)
    c0 = np.clip(qc - 8, 0, 48)
    colok = (kc[:, None] >= c0[None, :]) & (kc[:, None] < c0[None, :] + 16)
    dcol = np.clip(kc[:, None] - qc[None, :] + 15, 0, 30)
    for m in (0, 1, 3):
        for j in na_key_tiles(m):
            ti = na_tile_index(m, j)
            for krl in range(2):
                kr = 2 * j + krl
                for qrl in range(8):
                    qr = 8 * m + qrl
                    r0 = min(max(qr - 4, 0), 24)
                    if not (r0 <= kr <= r0 + 7):
                        continue
                    drow = kr - qr + 7
                    blk = rpb[:, drow][:, dcol]
                    blk = np.where(colok[None], blk, np.float32(NEG))
                    out[:, ti, krl * 64:(krl + 1) * 64, qrl * 64:(qrl + 1) * 64] = blk
    return out


def prepare_inputs(inputs):
    f = lambda k: np.asarray(inputs[k], np.float32)
    sh = {}
    sh["ada_w"] = f("ada_w")
    sh["adab"] = np.ascontiguousarray(f("ada_b").reshape(2, 48, 128).transpose(2, 0, 1))
    nm = f("norm_mix_g").reshape(2, KC, 128)
    nf = f("norm_ffn_g").reshape(2, KC, 128)
    sh["nrm_g"] = np.ascontiguousarray(np.stack([nm, nf], 1).transpose(3, 0, 1, 2))
    w_in0 = f("even_w_in")[0]
    sh["w_in0"] = w_in0
    sh["w_out0"] = f("even_w_out")[0]
    wg = np.zeros((D, 128), np.float32)
    g = w_in0[:, 3072:3088]
    wg[:, 0:4] = g[:, 0:4]
    wg[:, 32:36] = g[:, 8:12]
    wg[:, 64:68] = g[:, 4:8]
    wg[:, 96:100] = g[:, 12:16]
    sh["wgate"] = wg
    gb = f("mlstm_gate_b")[0]
    gateb = np.zeros((128, 2), np.float32)
    gateb[0:4, 0] = gb[0:4]
    gateb[32:36, 0] = gb[8:12]
    gateb[0:4, 1] = gb[4:8]
    gateb[32:36, 1] = gb[12:16]
    sh["gateb"] = gateb
    sh["na_g"] = np.ascontiguousarray(np.stack([np.tile(f("na_q_norm_g")[0], 2), np.tile(f("na_k_norm_g")[0], 2)], 1))
    sh["btab"] = _na_bias_tables(f("na_rpb")[0])
    sh["ml_g"] = np.ascontiguousarray(np.broadcast_to(f("mlstm_norm_g")[0][None, :], (128, 512)))
    perm = np.concatenate([np.arange(0, 128, 2), np.arange(1, 128, 2)])
    w_in1 = f("odd_w_in")[0].copy()
    for h in range(10):
        w_in1[:, h * 128:(h + 1) * 128] = w_in1[:, h * 128:(h + 1) * 128][:, perm]
    sh["w_in1"] = w_in1
    sh["w_out1"] = f("odd_w_out")[0]
    sh["gqa_g"] = np.ascontiguousarray(np.stack([f("gqa_q_norm_g")[0][perm], f("gqa_k_norm_g")[0][perm]], 1))
    C, S = _rope_tables()
    sh["ropeC"], sh["ropeS"] = C, S
    sh["router_w"] = f("router_w")
    sh["router_b"] = np.ascontiguousarray(np.broadcast_to(f("router_b")[None, :], (128, NE)))
    sh["exp_w1"], sh["exp_w3"], sh["exp_w2"] = f("exp_w1"), f("exp_w3"), f("exp_w2")
    sh["consts"] = _const_tables()
    x, ctx, c, c_ctx = f("x"), f("ctx"), f("c"), f("c_ctx")
    per = []
    for b in range(x.shape[0]):
        d = dict(sh)
        d["xT"] = np.ascontiguousarray(np.concatenate([ctx[b], x[b]], 0).T)
        d["cvec"] = np.ascontiguousarray(np.stack([_fm(c[b]), _fm(c_ctx)], 2))
        per.append(d)
    return per


_CACHE = {}


def kernel(**inputs):
    per = prepare_inputs(inputs)
    if "nc" not in _CACHE:
        _CACHE["nc"] = build_program()[0]
    nc = _CACHE["nc"]
    res = run_bass_kernel_spmd(nc, per, core_ids=list(range(len(per))))
    out = np.stack([np.ascontiguousarray(r["outT"].T) for r in res.results], 0)
    return out.astype(np.float32)
```

```python
import contextlib
import os
import numpy as np
import concourse.bass as bass
import concourse.mybir as mybir
from concourse.bass_utils import run_bass_kernel_spmd

F32 = mybir.dt.float32
BF16 = mybir.dt.bfloat16
AF = mybir.ActivationFunctionType
ALU = mybir.AluOpType
AX = mybir.AxisListType

D = 1024
L_CTX = 256
S_LAT = 2048
T = L_CTX + S_LAT
NT = T // 128
KC = D // 128
EPS = 1e-6
NE = 16
DFF = 512
NEG = -30000.0
TT = [(0, 256), (256, 512), (768, 512), (1280, 512), (1792, 512)]

ENGS = ("pe", "act", "dve", "pool", "sp")


class Instr:
    __slots__ = ("eng", "fn", "dma", "deps", "signal", "sig", "sem", "target")

    def __init__(self, eng, fn, dma):
        self.eng = eng
        self.fn = fn
        self.dma = dma
        self.deps = ()
        self.signal = False
        self.sig = 0
        self.sem = None
        self.target = 0


class Prog:
    def __init__(self):
        self.instrs = {e: [] for e in ENGS}
        self.wr = {}
        self.wr_dma = {}
        self.rd = {}
        self.rd_dma = {}
        self.pending = {e: [] for e in ENGS}

    def fence(self):
        lasts = [self.instrs[e][-1] for e in ("pe", "act", "dve", "pool") if self.instrs[e]]
        lasts += [i for i in self.instrs["sp"] if i.dma][-24:]
        for e in ENGS:
            self.pending[e] = list(lasts)

    def add(self, eng, fn, reads=(), writes=(), dma=False):
        ins = Instr(eng, fn, dma)
        pr = [k for k in reads if k[0] in ("ps", "psb")]
        if pr:
            reads = [k for k in reads if k[0] not in ("ps", "psb")]
            writes = list(writes) + pr
        deps = {}
        for k in reads:
            w = self.wr.get(k)
            if w:
                for d in w.values():
                    deps[id(d)] = d
            d = self.wr_dma.get(k)
            if d is not None:
                deps[id(d)] = d
        for k in writes:
            w = self.wr.get(k)
            if w:
                for d in w.values():
                    deps[id(d)] = d
            d = self.wr_dma.get(k)
            if d is not None:
                deps[id(d)] = d
            r = self.rd.get(k)
            if r:
                for d in r.values():
                    deps[id(d)] = d
            for d in self.rd_dma.get(k, ()):
                deps[id(d)] = d
        for d in self.pending[eng]:
            deps[id(d)] = d
        self.pending[eng] = []
        ins.deps = [d for d in deps.values() if d is not ins and (d.dma or d.eng != eng or dma or eng != "pe")]
        for d in ins.deps:
            d.signal = True
        for k in reads:
            if dma:
                self.rd_dma.setdefault(k, []).append(ins)
            else:
                self.rd.setdefault(k, {})[eng] = ins
        for k in writes:
            if dma:
                self.wr_dma[k] = ins
                self.wr[k] = {}
            else:
                self.wr.setdefault(k, {})[eng] = ins
                self.wr_dma[k] = None
            self.rd[k] = {}
            self.rd_dma[k] = []
        self.instrs[eng].append(ins)
        return ins

    def emit(self, nc, es, n_dma_sems=24):
        handles = {"pe": nc.tensor, "act": nc.scalar, "dve": nc.vector, "pool": nc.gpsimd, "sp": nc.sync}
        esem = {e: es.enter_context(nc.semaphore("c_" + e)) for e in ENGS}
        dsems = {e: [es.enter_context(nc.semaphore("d_%s%d" % (e, i))) for i in range(n_dma_sems)]
                 for e in ("sp",)}
        prev_dma = {}
        for e in ENGS:
            idx = 0
            nd = 0
            for ins in self.instrs[e]:
                if ins.dma:
                    pool = dsems[e]
                    ins.sem = pool[nd % len(pool)]
                    ins.target = 16 * (nd // len(pool) + 1)
                    nd += 1
                elif ins.signal:
                    idx += 1
                    ins.sig = idx
        block = es.enter_context(nc.Block())

        def body(e):
            def run(eng):
                waited = {}
                for ins in self.instrs[e]:
                    for d in ins.deps:
                        if d.dma:
                            sem, val = d.sem, d.target
                        else:
                            sem, val = esem[d.eng], d.sig
                        key = id(sem)
                        if waited.get(key, 0) >= val:
                            continue
                        eng.wait_ge(sem, val)
                        waited[key] = val
                    if ins.dma and ins.target > 16:
                        key = id(ins.sem)
                        if waited.get(key, 0) < ins.target - 16:
                            eng.wait_ge(ins.sem, ins.target - 16)
                            waited[key] = ins.target - 16
                    r = ins.fn(eng)
                    if ins.dma:
                        r.then_inc(ins.sem, 16)
                    elif ins.signal:
                        r.then_inc(esem[e], 1)
            return run

        block.sync(body("sp"))
        block.tensor(body("pe"))
        block.scalar(body("act"))
        block.vector(body("dve"))
        block.gpsimd(body("pool"))


def bkeys(name, t0, n):
    return [(name, b) for b in range(t0 // 128, (t0 + n + 127) // 128)]


class K:
    pass


def build_program(stop=None, debug=0):
    nc = bass.Bass("TRN2", target_bir_lowering=False)
    P = Prog()
    es = contextlib.ExitStack()
    es.__enter__()

    def dram_in(name, shape, dt=F32):
        return nc.dram_tensor(name, list(shape), dt, kind="ExternalInput").ap()

    xT = dram_in("xT", [D, T])
    cvec = dram_in("cvec", [128, KC, 2])
    ada_w = dram_in("ada_w", [2, D, 6 * D])
    adab = dram_in("adab", [128, 2, 48])
    nrm_g = dram_in("nrm_g", [128, 2, 2, KC])
    w_in0 = dram_in("w_in0", [D, 3088])
    w_out0 = dram_in("w_out0", [D, D])
    wgate = dram_in("wgate", [D, 128])
    gateb = dram_in("gateb", [128, 2])
    na_g = dram_in("na_g", [128, 2])
    btab = dram_in("btab", [8, 20, 128, 512])
    ml_g = dram_in("ml_g", [128, 512])
    w_in1 = dram_in("w_in1", [D, 1536])
    w_out1 = dram_in("w_out1", [D, D])
    gqa_g = dram_in("gqa_g", [128, 2])
    ropeC = dram_in("ropeC", [128, S_LAT])
    ropeS = dram_in("ropeS", [128, S_LAT])
    router_w = dram_in("router_w", [D, NE])
    router_b = dram_in("router_b", [128, NE])
    exp_w1 = dram_in("exp_w1", [2, NE, D, DFF])
    exp_w3 = dram_in("exp_w3", [2, NE, D, DFF])
    exp_w2 = dram_in("exp_w2", [2, NE, DFF, D])
    consts = dram_in("consts", [128, 1024])
    outT = nc.dram_tensor("outT", [D, S_LAT], F32, kind="ExternalOutput").ap()
    xsp = nc.dram_tensor("xsp", [D, T], F32, kind="Internal").ap()
    dbg = None
    if debug:
        dbg = nc.dram_tensor("dbg", [128, debug], F32, kind="ExternalOutput").ap()

    def sb(name, shape, dt):
        return es.enter_context(nc.sbuf_tensor(name, list(shape), dt))

    XR = sb("XR", [128, KC * T], F32)
    HT = sb("HT", [128, KC, T], BF16)
    MIX = sb("MIX", [128, KC * T], BF16)
    SCR = sb("SCR", [128, 9216], F32)
    CST = sb("CST", [128, 1024], F32)
    CSTB = sb("CSTB", [128, 1024], BF16)
    MOD = sb("MOD", [128, 2, 48, 2], F32)
    GS = sb("GS", [128, 2, 2, KC, 2], F32)
    SML = sb("SML", [128, 256], F32)
    PS = [es.enter_context(nc.psum_tensor("ps%d" % i, [128, 512], F32)) for i in range(7)]
    PSB = es.enter_context(nc.psum_tensor("psb", [128, 1024], BF16))

    X = XR[:].rearrange("p (k t) -> p k t", k=KC)
    k_ = K()
    k_.nc, k_.P = nc, P

    ident = CST[:, 0:128]
    ones_f = CST[:, 128:256]
    identb = CSTB[:, 0:128]
    bd64b = CSTB[:, 768:896]
    trif = CST[:, 256:384]
    trib = CST[:, 384:512]
    swapb = CSTB[:, 512:640]
    onesb = CSTB[:, 640:768]

    def dma(out, in_, reads, writes, eng="sp"):
        return P.add(eng, lambda e: e.dma_start(out=out, in_=in_), reads, writes, dma=True)

    def mm(out, lhsT, rhs, start, stop, reads, writes):
        return P.add("pe", lambda e: e.matmul(out, lhsT=lhsT, rhs=rhs, start=start, stop=stop), reads, writes)

    def tr(out, in_, idn, reads, writes):
        return P.add("pe", lambda e: e.transpose(out=out, in_=in_, identity=idn), reads, writes)

    def act(out, in_, func, reads, writes, scale=None, bias=None):
        kw = {}
        if scale is not None:
            kw["scale"] = scale
        if bias is not None:
            kw["bias"] = bias
        return P.add("act", lambda e: e.activation(out=out, in_=in_, func=func, **kw), reads, writes)

    def tt(out, in0, in1, op, reads, writes, eng="dve"):
        return P.add(eng, lambda e: e.tensor_tensor(out=out, in0=in0, in1=in1, op=op), reads, writes)

    def ts(out, in0, s1, s2, op0, op1, reads, writes, eng="dve"):
        if op1 is None:
            return P.add(eng, lambda e: e.tensor_scalar(out=out, in0=in0, scalar1=s1, scalar2=None, op0=op0), reads, writes)
        return P.add(eng, lambda e: e.tensor_scalar(out=out, in0=in0, scalar1=s1, scalar2=s2, op0=op0, op1=op1), reads, writes)

    def stt(out, in0, scalar, in1, op0, op1, reads, writes):
        return P.add("dve", lambda e: e.scalar_tensor_tensor(out=out, in0=in0, scalar=scalar, in1=in1, op0=op0, op1=op1),
                     reads, writes)

    def cp(out, in_, reads, writes, eng="dve"):
        if eng == "act":
            return P.add(eng, lambda e: e.activation(out=out, in_=in_, func=AF.Copy), reads, writes)
        return P.add(eng, lambda e: e.tensor_copy(out=out, in_=in_), reads, writes)

    def memset(ap, val, writes, eng="pool"):
        return P.add(eng, lambda e: e.memset(ap, val), (), writes)

    def red(out, in_, op, reads, writes, axis=AX.X):
        return P.add("dve", lambda e: e.tensor_reduce(out=out, in_=in_, axis=axis, op=op), reads, writes)

    def recip(out, in_, reads, writes):
        return P.add("dve", lambda e: e.reciprocal(out=out, in_=in_), reads, writes)

    dbg_off = [0]
    cnt = {"nm": 0, "ada": 0}

    def tap(ap, n, reads):
        if dbg is None or debug == 1:
            return
        o = dbg_off[0]
        dbg_off[0] += n
        k_.dbg_slots.append((o, n))
        stg = k_.dbg_stage
        cp(stg[:, 0:n], ap, reads + [("dbgstage",)], [("dbgstage",)])
        dma(dbg[:, o:o + n], stg[:, 0:n], [("dbgstage",)], [("dbg", o)])
        k_.out_keys.append(("dbg", o))

    k_.dbg_slots = []
    k_.out_keys = []
    k_.dbg_stage = None
    if dbg is not None and debug > 1:
        k_.dbg_stage = sb("dbgst", [128, 2304], F32)

    def finish():
        P.add("sp", lambda e: e.nop(), list(k_.out_keys), [("final",)])
        P.emit(nc, es)
        es.close()
        return nc, k_

    dma(CST[:], consts[:, :], [], [("CST",)])
    cp(CSTB[:], CST[:], [("CST",)], [("CSTB",)], eng="dve")
    dma(SML[:, 0:2], gateb[:, :], [], [("gateb",)])
    dma(SML[:, 2:4], na_g[:, :], [], [("na_g",)])
    dma(SML[:, 4:6], gqa_g[:, :], [], [("gqa_g",)])
    dma(SML[:, 16:32], router_b[:, :], [], [("router_b",)])
    NRM = sb("NRM", [128, 2, 2, KC], F32)
    dma(NRM[:], nrm_g[:, :, :, :], [], [("NRM",)])
    ADB = sb("ADB", [128, 2, 48], F32)
    dma(ADB[:], adab[:, :, :], [], [("ADB",)])
    CV = sb("CV", [128, KC, 2], F32)
    dma(CV[:], cvec[:, :, :], [], [("CV",)])
    SCV = sb("SCV", [128, KC, 2], F32)
    act(SCV[:], CV[:], AF.Silu, [("CV",)], [("SCV",)])

    def ada_gs(l, w):
        base = 8 if w == 0 else 32
        ts(GS[:, l, w], MOD[:, l, base:base + 8, :], 1.0, None, ALU.add, None, [("MOD", l)], [("GS", l, w)])
        tt(GS[:, l, w], GS[:, l, w], NRM[:, l, w].unsqueeze(2).to_broadcast([128, KC, 2]), ALU.mult,
           [("GS", l, w), ("NRM",)], [("GS", l, w)])

    def ada_block(l, j0, nch, stage, skey, ps, pkey):
        aw = ada_w[l].rearrange("(k p) c -> p k c", p=128)
        dma(stage, aw[:, :, j0 * 128:(j0 + nch) * 128], [], [skey])
        for jj in range(nch):
            for kc in range(KC):
                mm(ps[:, 2 * jj:2 * jj + 2], stage[:, kc, jj * 128:(jj + 1) * 128], SCV[:, kc, :],
                   kc == 0, kc == KC - 1, [skey, ("SCV",)], [pkey])
        tt(MOD[:, l, j0:j0 + nch, :], ps[:, 0:2 * nch].rearrange("p (j t) -> p j t", t=2),
           ADB[:, l, j0:j0 + nch].unsqueeze(2).to_broadcast([128, nch, 2]), ALU.add, [pkey, ("ADB",)], [("MOD", l)])

    stage0 = XR[:, 0:8192].rearrange("p (s k c) -> p s k c", s=2, k=KC)
    for blk in range(4):
        ada_block(0, blk * 4, 4, stage0[:, blk % 2], ("adast", blk % 2), PS[6], ("ps", 6))
    ada_gs(0, 0)
    ada_list = [(0, j) for j in range(16, 48)] + [(1, j) for j in range(48)]
    PSBf = PSB[:].bitcast(F32)

    SCVb = sb("SCVb", [128, KC, 2], BF16)
    cp(SCVb[:], SCV[:], [("SCV",)], [("SCVb",)])
    MT = sb("MT", [2, 256], F32)

    ABW4 = MIX[:, 9216:13312].rearrange("p (s k c) -> p s k c", s=4, k=KC)
    issued = [0]

    def ada_bg_dma(upto):
        while issued[0] < min(upto, len(ada_list)):
            idx = issued[0]
            issued[0] += 1
            l_, j = ada_list[idx]
            k = idx % 4
            stg = SCR[:, 4608 + k * 1024:4608 + (k + 1) * 1024].rearrange("p (k c) -> p k c", k=KC)
            aw = ada_w[l_].rearrange("(k p) c -> p k c", p=128)
            dma(stg, aw[:, :, j * 128:(j + 1) * 128], [], [("adabg", k)])
            cp(ABW4[:, k], stg, [("adabg", k)], [("abw", k)], eng="pool")

    def ada_bg():
        idx = cnt["ada"]
        if idx >= len(ada_list):
            return
        cnt["ada"] += 1
        ada_bg_dma(idx + 4)
        l_, j = ada_list[idx]
        k = idx % 4
        c2 = 2 * (idx % 2)
        for kc in range(KC):
            mm(PSBf[:, c2:c2 + 2], ABW4[:, k, kc, :], SCVb[:, kc, :], kc == 0, kc == KC - 1, [("SCVb",), ("abw", k)], [("psb",)])
        tt(MOD[:, l_, j, :], PSBf[:, c2:c2 + 2], ADB[:, l_, j:j + 1].to_broadcast([128, 2]), ALU.add, [("psb",), ("ADB",)], [("MOD", l_)])

    def ada_finish():
        while cnt["ada"] < len(ada_list):
            ada_bg()
        ada_gs(0, 1)
        ada_gs(1, 0)
        ada_gs(1, 1)

    if stop == "ada":
        ada_finish()
        tap(MOD[:, 0].rearrange("p j t -> p (j t)"), 96, [("MOD", 0)])
        tap(MOD[:, 1].rearrange("p j t -> p (j t)"), 96, [("MOD", 1)])
        return finish()

    def norm_modulate(l, w, src_tile, tiles, h32_cb=None):
        sh_base = 0 if w == 0 else 24
        SCRb_ = SCR[:].bitcast(BF16)
        for i in tiles:
            t0, n = TT[i]
            seg = 1 if i == 0 else 0
            xa, xk = src_tile(i)
            s2 = cnt["nm"] % 2
            cnt["nm"] += 1
            sq = SCR[:, s2 * 4096:(s2 + 1) * 4096].rearrange("p (k t) -> p k t", k=KC)[:, :, 0:n]
            sqb = SCRb_[:, 16384 + s2 * 1024:16384 + (s2 + 1) * 1024]
            sqb = SCRb_[:, s2 * 8192:s2 * 8192 + 4096].rearrange("p (k t) -> p k t", k=KC)[:, :, 0:n]
            act(sqb, xa, AF.Square, xk + [("sq", s2)], [("sq", s2)])
            ps = PS[5]
            for kc in range(KC):
                mm(ps[:, 0:n], onesb, sqb[:, kc, :], kc == 0, kc == KC - 1, [("sq", s2), ("CSTB",)], [("ps", 5)])
            rstd = SCR[:, 8192 + s2 * 512:8192 + (s2 + 1) * 512][:, 0:n]
            act(rstd, ps[:, 0:n], AF.Ln, [("ps", 5)], [("rstd", s2)], scale=1.0 / D, bias=EPS)
            act(rstd, rstd, AF.Exp, [("rstd", s2)], [("rstd", s2)], scale=-0.5)
            tt(sq, xa, rstd.unsqueeze(1).to_broadcast([128, KC, n]), ALU.mult, xk + [("rstd", s2), ("sq", s2)], [("sq", s2)])
            for kc in range(KC):
                ts(HT[:, kc, t0:t0 + n], sq[:, kc, :], GS[:, l, w, kc, seg:seg + 1], MOD[:, l, sh_base + kc, seg:seg + 1],
                   ALU.mult, ALU.add, [("sq", s2), ("GS", l, w), ("MOD", l)], bkeys("HT", t0, n))
            if h32_cb is not None:
                h32_cb(i, sq, s2, t0, n)

    xv = xT.rearrange("(k p) t -> p k t", p=128)
    XST = XR[:, 8192:16384].rearrange("p (s k t) -> p s k t", s=2, k=KC)

    def src_dram(view):
        def f(i):
            t0, n = TT[i]
            s = i % 2
            dma(XST[:, s, :, 0:n], view[:, :, t0:t0 + n], [], [("xst", s)])
            return XST[:, s, :, 0:n], [("xst", s)]
        return f

    norm_modulate(0, 0, src_dram(xv), range(5))
    if stop == "h0dbg":
        return finish()
    if stop == "h0":
        for kc in range(KC):
            tap(HT[:, kc, :], T, bkeys("HT", 0, T))
        return finish()

    def fence(tag):
        P.fence()

    XRb = XR[:].bitcast(BF16)
    SCRb = SCR[:].bitcast(BF16)
    MIXv = MIX[:].rearrange("p (k t) -> p k t", k=KC)
    wv0 = w_in0.rearrange("(k p) c -> p k c", p=128)
    cnt.update({"st": 0, "po": 0, "pt": 0, "bt": 0, "sbt": 0, "pj": 0})

    fence("na")
    if stop == "fence":
        tap(HT[:, 0, :], T, bkeys("HT", 0, T))
        return finish()
    NAGs = SML[:, 8:10]
    ts(NAGs[:, 0:1], SML[:, 2:3], 0.125, None, ALU.mult, None, [("na_g",)], [("nags",)])
    cp(NAGs[:, 1:2], SML[:, 3:4], [("na_g",), ("nags",)], [("nags",)])
    for ub in range(2):
        VAb = XRb[:, ub * 9216 + 4608:ub * 9216 + 9216].rearrange("p (t h c) -> p t h c", t=NT, h=2)
        memset(VAb[:, :, 0, 64:128], 1.0, [("nav", ub)])
        memset(VAb[:, :, 1, 0:64], 1.0, [("nav", ub)])
    if stop == "fence2":
        tap(XRb[:, 4608:4608 + 2304], T, [("nav", 0)])
        return finish()
    PTr = SCRb[:, 0:2048].rearrange("p (s t) -> p s t", s=4)
    RC = SCR[:, 1024:1536]
    SQN = SCRb[:, 3072:3584]
    RSTD = SCR[:, 2048:2560]
    BTr = XR[:, 15360:17408].rearrange("p (s t) -> p s t", s=4)
    SBr = XR[:, 17408:18432].rearrange("p (s t) -> p s t", s=2)

    def na_views(u):
        ub = u % 2
        base = ub * 9216
        qT = XRb[:, base:base + 2304]
        kT = XRb[:, base + 2304:base + 4608]
        VA = XRb[:, base + 4608:base + 9216].rearrange("p (t h c) -> p t h c", t=NT, h=2)
        W = XRb[:, 18432 + ub * 3072:18432 + (ub + 1) * 3072].rearrange("p (k c) -> p k c", k=KC)
        return ub, qT, kT, VA, W

    def na_proj_stages(u):
        ub, qT, kT, VA, W = na_views(u)
        WS = XR[:, 12288:15360].rearrange("p (k c) -> p k c", k=KC)
        st = []

        def s_load():
            for i, c0 in enumerate((u * 128, 512 + u * 128, 1024 + u * 128)):
                dma(WS[:, :, i * 128:(i + 1) * 128], wv0[:, :, c0:c0 + 128], [], [("naws", i)])
                cp(W[:, :, i * 128:(i + 1) * 128], WS[:, :, i * 128:(i + 1) * 128], [("naws", i)], [("naw", ub, i)], eng="pool")
        st.append(s_load)
        st.append(lambda: None)
        st.append(lambda: None)
        for which, dst, dkey in ((0, qT, "naq"), (1, kT, "nak")):
            for i in range(5):
                t0, n = TT[i]
                pi = cnt["pj"] % 2
                cnt["pj"] += 1

                def s0(which=which, t0=t0, n=n, pi=pi):
                    for kc in range(KC):
                        mm(PS[pi][:, 0:n], W[:, kc, which * 128:(which + 1) * 128], HT[:, kc, t0:t0 + n], kc == 0, kc == KC - 1,
                           [("naw", ub, which)] + bkeys("HT", t0, n), [("ps", pi)])

                def s1(n=n, pi=pi):
                    act(SQN[:, 0:n], PS[pi][:, 0:n], AF.Square, [("ps", pi)], [("sqn",)])

                def s2(n=n):
                    mm(PS[2][:, 0:n], bd64b, SQN[:, 0:n], True, True, [("sqn",), ("CSTB",)], [("ps", 2)])

                def s3(n=n):
                    act(RSTD[:, 0:n], PS[2][:, 0:n], AF.Ln, [("ps", 2)], [("rstd",)], scale=1.0 / 64, bias=EPS)
                    act(RSTD[:, 0:n], RSTD[:, 0:n], AF.Exp, [("rstd",)], [("rstd",)], scale=-0.5)

                def s4(which=which, dst=dst, dkey=dkey, t0=t0, n=n, pi=pi, i=i):
                    stt(dst[:, t0:t0 + n], PS[pi][:, 0:n], NAGs[:, which:which + 1], RSTD[:, 0:n], ALU.mult, ALU.mult,
                        [("ps", pi), ("rstd",), ("nags",)], [(dkey, ub, i)])
                st += [s0, s1, s2, s3, s4]
        for g in range(5):
            nb = 4 if g < 4 else 2
            pi = cnt["pj"] % 2
            cnt["pj"] += 1

            def v0(g=g, nb=nb, pi=pi):
                for b4 in range(nb):
                    tb = g * 4 + b4
                    for kc in range(KC):
                        mm(PS[pi][:, b4 * 128:(b4 + 1) * 128], HT[:, kc, tb * 128:(tb + 1) * 128], W[:, kc, 256:384], kc == 0, kc == KC - 1,
                           [("naw", ub, 2), ("HT", tb)], [("ps", pi)])

            def v1(g=g, nb=nb, pi=pi):
                pv = PS[pi][:, 0:nb * 128].rearrange("p (b c) -> p b c", c=128)
                cp(VA[:, g * 4:g * 4 + nb, 0, 0:64], pv[:, :, 0:64], [("ps", pi)], [("nav", ub)], eng="act")
                cp(VA[:, g * 4:g * 4 + nb, 1, 64:128], pv[:, :, 64:128], [("ps", pi)], [("nav", ub)], eng="dve")
            st += [v0, v1]
        return st

    def na_unit(u, bg):
        ub, qT, kT, VA, W = na_views(u)
        if stop == "naproj":
            tap(qT, T, [("naq", ub, i) for i in range(5)])
            tap(kT, T, [("nak", ub, i) for i in range(5)])
            for tb in range(NT):
                tap(VA[:, tb, 0, :], 128, [("nav", ub)])
            return
        its = []
        for hh in range(2):
            for blk in range(5):
                if blk == 0:
                    q0, nq, qi = 0, 256, 0
                    keyt = [(0, None), (1, None)]
                else:
                    m = blk - 1
                    q0, nq, qi = 256 + m * 512, 512, blk
                    keyt = [(0, None), (1, None)] + [(2 + j, na_tile_index(m, j)) for j in na_key_tiles(m)]
                for ki, (kt, bi) in enumerate(keyt):
                    its.append((hh, blk, q0, nq, qi, ki, len(keyt), kt, bi))

        def st_mm(x):
            hh, blk, q0, nq, qi, ki, nk, kt, bi = its[x]
            p0 = hh * 64
            si = 3 + x % 2
            kti = 0 if kt < 2 else 1 + (kt - 2) // 4
            mm(PS[si][:, 0:nq], kT[p0:p0 + 64, kt * 128:(kt + 1) * 128], qT[p0:p0 + 64, q0:q0 + nq], True, True,
               [("nak", ub, kti), ("naq", ub, qi)], [("ps", si)])

        st_mm(0)
        nblk = 0
        for x, (hh, blk, q0, nq, qi, ki, nk, kt, bi) in enumerate(its):
            h = 2 * u + hh
            p0 = hh * 64
            dn = 64 - p0
            si = 3 + x % 2
            pst = PS[si]
            if ki == 0:
                oi = 5 + cnt["po"] % 2
                cnt["po"] += 1
            po = PS[oi]
            if x + 1 < len(its):
                st_mm(x + 1)
            pti = cnt["pt"] % 4
            cnt["pt"] += 1
            if bi is None:
                act(PTr[:, pti, 0:nq], pst[:, 0:nq], AF.Exp, [("ps", si)], [("pt", pti)])
            else:
                bti = cnt["bt"] % 4
                cnt["bt"] += 1
                dma(BTr[:, bti, :], btab[h, bi], [], [("bt", bti)])
                sbi = cnt["sbt"] % 2
                cnt["sbt"] += 1
                tt(SBr[:, sbi, :], pst[:, 0:nq], BTr[:, bti, :], ALU.add, [("ps", si), ("bt", bti)], [("sbt", sbi)])
                act(PTr[:, pti, 0:nq], SBr[:, sbi, :], AF.Exp, [("sbt", sbi)], [("pt", pti)])
            mm(po[:, 0:nq], VA[:, kt, hh, :], PTr[:, pti, 0:nq], ki == 0, ki == nk - 1,
               [("nav", ub), ("pt", pti)], [("ps", oi)])
            if x % 3 == 1:
                ada_bg()
            if bg:
                bg.pop(0)()
            if ki == nk - 1:
                act(RC[dn:dn + 64, 0:nq], po[dn:dn + 64, 0:nq], AF.Ln, [("ps", oi)], [("rc",)])
                act(RC[dn:dn + 64, 0:nq], RC[dn:dn + 64, 0:nq], AF.Exp, [("rc",)], [("rc",)], scale=-1.0)
                tt(MIXv[p0:p0 + 64, u, q0:q0 + nq], po[p0:p0 + 64, 0:nq], RC[dn:dn + 64, 0:nq], ALU.mult,
                   [("ps", oi), ("rc",)], [("MIX", u, blk)])

    for f in na_proj_stages(0):
        f()
    for u in range(4):
        bg = na_proj_stages(u + 1) if u + 1 < 4 else []
        na_unit(u, bg)
        while bg:
            bg.pop(0)()
        if stop == "naproj":
            return finish()
    ada_finish()
    if stop == "na":
        for u in range(4):
            tap(MIXv[:, u, :], T, [("MIX", u, b) for b in range(5)])
        return finish()

    fence("ml")
    T1 = XR[:, 0:2304]
    T2 = XR[:, 2304:4608]
    T3 = XR[:, 4608:6912]
    T4 = XR[:, 6912:9216]
    WGs = XR[:, 9216:10240].rearrange("p (k c) -> p k c", k=KC)
    WGb = XRb[:, 20480:21504].rearrange("p (k c) -> p k c", k=KC)
    KWT = SCR[:, 4608:5040].rearrange("p (t q r) -> p t q r", t=NT, q=3)
    MC = SCR[:, 5040:5058]
    MN = SCR[:, 5058:5076]
    SCN = SCR[:, 5076:5094]
    NEGB = SCR[:, 5094:5095]
    ZZ = SCR[:, 5096:5240].rearrange("p (r c) -> p r c", r=8)
    SCB = SCR[:, 5240:5384]
    selr = CST[:, 896:904]
    dma(WGs, wgate.rearrange("(k p) c -> p k c", p=128), [], [("wgs",)])
    cp(WGb, WGs, [("wgs",)], [("wgb",)], eng="pool")
    ts(NEGB[0:64, :], SML[0:64, 1:2], -1.0, None, ALU.mult, None, [("gateb",)], [("negb",)])
    memset(T2[0:64, :], 0.0, [("T2",)], eng="pool")
    memset(MC[0:64, :], 0.0, [("MC",)], eng="pool")
    memset(MN[0:64, :], 0.0, [("MN",)], eng="pool")
    for i in range(5):
        t0, n = TT[i]
        for kc in range(KC):
            mm(PS[0][0:64, 0:n], WGb[:, kc, 0:64], HT[:, kc, t0:t0 + n], kc == 0, kc == KC - 1, [("wgb",)] + bkeys("HT", t0, n), [("ps", 0)])
        for kc in range(KC):
            mm(PS[1][0:64, 0:n], WGb[:, kc, 64:128], HT[:, kc, t0:t0 + n], kc == 0, kc == KC - 1, [("wgb",)] + bkeys("HT", t0, n), [("ps", 1)])
        ts(T3[0:64, t0:t0 + n], PS[0][0:64, 0:n], SML[0:64, 0:1], None, ALU.add, None, [("ps", 0), ("gateb",)], [("T3",)])
        act(T1[0:64, t0:t0 + n], PS[1][0:64, 0:n], AF.Exp, [("ps", 1), ("negb",)], [("T1",)], scale=-1.0, bias=NEGB[0:64, 0:1])
    act(T1[0:64, :], T1[0:64, :], AF.Ln, [("T1",)], [("T1",)], bias=1.0)

    def scan(out, d0, d1, init, op0, op1, reads, writes):
        return P.add("dve", lambda e: e.tensor_tensor_scan(out=out, data0=d0, data1=d1, initial=init, op0=op0, op1=op1), reads, writes)

    def ones_b(p0, n):
        return ones_f[p0:p0 + 4, 0:1].to_broadcast([4, n])

    scan(T2[0:4, :], ones_b(0, T), T1[0:4, :], 0.0, ALU.mult, ALU.add, [("T1",), ("CST",), ("T2",)], [("T2",)])
    scan(T2[32:36, 0:256][:, ::-1], ones_b(32, 256), T1[32:36, 0:256][:, ::-1], 0.0, ALU.mult, ALU.add, [("T1",), ("CST",), ("T2",)], [("T2",)])
    scan(T2[32:36, 256:T][:, ::-1], ones_b(32, S_LAT), T1[32:36, 256:T][:, ::-1], T2[32:36, 0:1], ALU.mult, ALU.add,
         [("T1",), ("CST",), ("T2",)], [("T2",)])
    tt(T3[0:64, :], T3[0:64, :], T2[0:64, :], ALU.add, [("T3",), ("T2",)], [("T3",)])
    scan(T4[0:4, :], T3[0:4, :], T3[0:4, :], -1e30, ALU.max, ALU.max, [("T3",)], [("T4",)])
    scan(T4[32:36, 0:256][:, ::-1], T3[32:36, 0:256][:, ::-1], T3[32:36, 0:256][:, ::-1], -1e30, ALU.max, ALU.max, [("T3",), ("T4",)], [("T4",)])
    scan(T4[32:36, 256:T][:, ::-1], T3[32:36, 256:T][:, ::-1], T3[32:36, 256:T][:, ::-1], T4[32:36, 0:1], ALU.max, ALU.max,
         [("T3",), ("T4",)], [("T4",)])
    U3 = lambda tl, a, b: tl[a:b, :].rearrange("p (c j) -> p c j", j=128)
    cp(MC[0:4, :], U3(T4, 0, 4)[:, :, 127], [("T4",), ("MC",)], [("MC",)])
    cp(MC[32:36, :], U3(T4, 32, 36)[:, :, 0], [("T4",), ("MC",)], [("MC",)])
    cp(MN[0:4, 0:17], MC[0:4, 1:18], [("MC",), ("MN",)], [("MN",)])
    cp(MN[0:4, 17:18], MC[0:4, 17:18], [("MC",), ("MN",)], [("MN",)])
    cp(MN[32:36, 1:2], MC[32:36, 0:1], [("MC",), ("MN",)], [("MN",)])
    cp(MN[32:36, 0:1], MC[32:36, 17:18], [("MC",), ("MN",)], [("MN",)])
    cp(MN[32:36, 3:18], MC[32:36, 2:17], [("MC",), ("MN",)], [("MN",)])
    cp(MN[32:36, 2:3], MC[32:36, 2:3], [("MC",), ("MN",)], [("MN",)])
    bc = lambda v: v[0:64, :].unsqueeze(2).to_broadcast([64, NT, 128])
    tt(U3(T1, 0, 64), U3(T3, 0, 64), bc(MC), ALU.subtract, [("T3",), ("MC",), ("T1",)], [("T1",)])
    act(T1[0:64, :], T1[0:64, :], AF.Exp, [("T1",)], [("T1",)])
    tt(U3(T3, 0, 64), U3(T3, 0, 64), bc(MN), ALU.subtract, [("T3",), ("MN",)], [("T3",)])
    act(T3[0:64, :], T3[0:64, :], AF.Exp, [("T3",)], [("T3",)])
    tt(U3(T2, 0, 64), U3(T2, 0, 64), bc(MC), ALU.subtract, [("T2",), ("MC",)], [("T2",)])
    act(T2[0:64, :], T2[0:64, :], AF.Exp, [("T2",)], [("T2",)])
    tt(SCN[0:64, :], MC[0:64, :], MN[0:64, :], ALU.subtract, [("MC",), ("MN",)], [("SCN",)])
    act(SCN[0:64, :], SCN[0:64, :], AF.Exp, [("SCN",)], [("SCN",)])
    tt(ZZ[0:64], SCN[0:64, :].unsqueeze(1).to_broadcast([64, 8, NT]), selr[0:64, :].unsqueeze(2).to_broadcast([64, 8, NT]), ALU.mult,
       [("SCN",), ("CST",)], [("ZZ",)])
    mm(PS[2][:, 0:144], ones_f[0:64, :], ZZ[0:64].rearrange("p r c -> p (r c)"), True, True, [("ZZ",), ("CST",)], [("ps", 2)])
    cp(SCB, PS[2][:, 0:144], [("ps", 2)], [("SCB",)])
    for tb in range(NT):
        pi = 3 + tb % 2
        for qi, Tq, key in ((0, T1, "T1"), (1, T3, "T3"), (2, T2, "T2")):
            tr(PS[pi][:, qi * 64:(qi + 1) * 64], Tq[0:64, tb * 128:(tb + 1) * 128], ident[0:64, 0:64], [(key,), ("CST",)], [("ps", pi)])
        pv = PS[pi][:, 0:192].rearrange("p (q r) -> p q r", r=64)
        cp(KWT[:, tb, :, 0:4], pv[:, :, 0:4], [("ps", pi)], [("KWT", tb)])
        cp(KWT[:, tb, :, 4:8], pv[:, :, 32:36], [("ps", pi)], [("KWT", tb)], eng="act")
    if stop == "mlscan":
        tap(KWT.rearrange("p t q r -> p (t q r)"), 432, [("KWT", tb) for tb in range(NT)])
        tap(SCB, 144, [("SCB",)])
        return finish()

    fence("mlu")
    mqT = XRb[:, 0:2304]
    mkT = XRb[:, 2304:4608]
    KT2 = XRb[:, 4608:9216].rearrange("p (t d c) -> p t d c", t=NT, d=2)
    MVA = XRb[:, 9216:13896].rearrange("p (t h c) -> p t h c", t=NT, h=2)
    MW = XRb[:, 13896:20040].rearrange("p (k c) -> p k c", k=KC)
    MWS = XR[:, 10240:12288].rearrange("p (k c) -> p k c", k=KC)
    HF = XR[:, 12288:16896].rearrange("p (t c) -> p t c", t=NT)
    SIG = SCRb[:, 0:4608].rearrange("p (t c) -> p t c", t=NT)
    PTm2 = SCRb[:, 4608:5120].rearrange("p (s h c) -> p s h c", s=2, h=2)
    XS = SCR[:, 5400:5658].rearrange("p (d c) -> p d c", d=2)
    XB = SCRb[:, 11400:11658].rearrange("p (d c) -> p d c", d=2)
    DD = SCR[:, 5900:5902]
    DN = SCR[:, 5904:5906]
    HS = SCR[:, 6000:6256]
    HQ = SCR[:, 6256:6512]
    SS = SCR[:, 6512:6514]
    HMb = SCRb[:, 14400:14656]
    MLG = SCR[:, 6656:7168]
    dma(MLG, ml_g[:, :], [], [("MLG",)])
    memset(MVA[:, :, :, 128:130], 1.0, [("mva",)])

    def ml_unit(u):
        pend_tr = []
        try:
            ml_unit_body(u, pend_tr)
        finally:
            pass
        while pend_tr:
            pend_tr.pop(0)()

    def ml_unit_body(u, pend_tr):
        pieces = ((0, 0, 1536 + u * 128, 128), (0, 128, 1792 + u * 128, 128), (1, 256, 2048 + u * 256, 256), (2, 512, 2560 + u * 256, 256))
        for pi_, (pc, wc0, c0, ncol) in enumerate(pieces):
            dma(MWS[:, :, 0:ncol], wv0[:, :, c0:c0 + ncol], [], [("mws",)])
            cp(MW[:, :, wc0:wc0 + ncol], MWS[:, :, 0:ncol], [("mws",)], [("mw", wc0)], eng="pool")
        for which, dst, dkey, scl in ((0, mqT, "mq", 0.125), (1, mkT, "mk", 1.0)):
            for i in range(5):
                t0, n = TT[i]
                pi = cnt["pj"] % 2
                cnt["pj"] += 1
                for kc in range(KC):
                    mm(PS[pi][:, 0:n], MW[:, kc, which * 128:(which + 1) * 128], HT[:, kc, t0:t0 + n], kc == 0, kc == KC - 1,
                       [("mw", which * 128)] + bkeys("HT", t0, n), [("ps", pi)])
                act(dst[:, t0:t0 + n], PS[pi][:, 0:n], AF.Copy, [("ps", pi)], [(dkey, i)], scale=scl)
        if stop == "mlA":
            tap(mqT, T, [("mq", i) for i in range(5)])
            return
        for tb in range(NT):
            pi = cnt["pj"] % 2
            cnt["pj"] += 1
            ps = PS[pi]
            for kc in range(KC):
                mm(ps[:, 0:128], HT[:, kc, tb * 128:(tb + 1) * 128], MW[:, kc, 128:256], kc == 0, kc == KC - 1, [("mw", 128), ("HT", tb)], [("ps", pi)])
            for kc in range(KC):
                mm(ps[:, 128:384], HT[:, kc, tb * 128:(tb + 1) * 128], MW[:, kc, 256:512], kc == 0, kc == KC - 1, [("mw", 256), ("HT", tb)], [("ps", pi)])
            for dr in range(2):
                if os.environ.get("SKIP") == "kt2":
                    break
                tt(KT2[:, tb, dr, :].rearrange("p (h c) -> p h c", h=2), ps[:, 0:128].rearrange("p (h c) -> p h c", h=2),
                   KWT[:, tb, 1, dr * 4 + 2 * u:dr * 4 + 2 * u + 2].unsqueeze(2).to_broadcast([128, 2, 64]), ALU.mult,
                   [("ps", pi), ("KWT", tb)], [("kt2", tb)])
            if os.environ.get("SKIP") != "mva":
                cp(MVA[:, tb, :, 0:128], ps[:, 128:384].rearrange("p (h c) -> p h c", h=2), [("ps", pi)], [("mva",)], eng="act")
            pi = cnt["pj"] % 2
            cnt["pj"] += 1
            ps = PS[pi]
            for kc in range(KC):
                mm(ps[:, 0:256], HT[:, kc, tb * 128:(tb + 1) * 128], MW[:, kc, 512:768], kc == 0, kc == KC - 1, [("mw", 512), ("HT", tb)], [("ps", pi)])
            if os.environ.get("SKIP") != "sig":
                act(SIG[:, tb, :], ps[:, 0:256], AF.Sigmoid, [("ps", pi)], [("sig", tb)])
        if stop == "mlB":
            tap(SIG[:, 0, :], 256, [("sig", 0)])
            tap(KT2[:, 0, 1, :], 128, [("kt2", 0)])
            tap(MVA[:, 3, 1, 0:129], 129, [("mva",)])
            return
        for dr in range(2):
            order = list(range(NT)) if dr == 0 else [1, 0] + list(range(17, 1, -1))
            if stop == "mlC":
                if dr == 1:
                    break
                order = order[:2]
            tri = trif if dr == 0 else trib
            def st_a(sx):
                cx = order[sx]
                cix = 0 if cx < 2 else 1 + (cx - 2) // 4
                cslx = slice(cx * 128, (cx + 1) * 128)
                for hh in range(2):
                    p0 = hh * 64
                    sbk = 2 if hh == 0 else 6
                    mm(PS[sbk][:, 0:128], mkT[p0:p0 + 64, cslx], mqT[p0:p0 + 64, cslx], True, True,
                       [("mk", cix), ("mq", cix)], [("ps", sbk)])
                for hh in range(2):
                    r = dr * 4 + 2 * u + hh
                    sbk = 2 if hh == 0 else 6
                    stt(PTm2[:, sx % 2, hh, :], PS[sbk][:, 0:128], KWT[:, cx, 0, r:r + 1], tri, ALU.mult, ALU.mult,
                        [("ps", sbk), ("KWT", cx), ("CST",)], [("ptm", sx % 2, hh)])

            st_a(0)
            for s_, c in enumerate(order):
                ci = 0 if c < 2 else 1 + (c - 2) // 4
                csl = slice(c * 128, (c + 1) * 128)
                if s_ + 1 < len(order):
                    st_a(s_ + 1)
                OB = (4, 5) if s_ % 2 == 0 else (0, 1)
                for hh in range(2):
                    p0 = hh * 64
                    mm(PS[3][p0:p0 + 64, 0:129], KT2[:, c, dr, hh * 64:(hh + 1) * 64], MVA[:, c, hh, 0:129], True, True,
                       [("kt2", c), ("mva",)], [("ps", 3)])
                for hh in range(2):
                    p0 = hh * 64
                    mm(PS[OB[hh]][:, 0:129], PTm2[:, s_ % 2, hh, :], MVA[:, c, hh, 0:129], True, s_ == 0,
                       [("ptm", s_ % 2, hh), ("mva",)], [("ps", OB[hh])])
                    if s_ > 0:
                        mm(PS[OB[hh]][:, 0:129], mqT[p0:p0 + 64, csl], XB[p0:p0 + 64, dr, :], False, True,
                           [("mq", ci), ("xb", hh)], [("ps", OB[hh])])
                while pend_tr:
                    pend_tr.pop(0)()
                for hh in range(2):
                    p0 = hh * 64
                    r = dr * 4 + 2 * u + hh
                    if s_ == 0:
                        cp(XS[p0:p0 + 64, dr, :], PS[3][p0:p0 + 64, 0:129], [("ps", 3)], [("xs", hh)])
                    else:
                        stt(XS[p0:p0 + 64, dr, :], XS[p0:p0 + 64, dr, :], SCB[p0:p0 + 64, r * NT + c:r * NT + c + 1], PS[3][p0:p0 + 64, 0:129],
                            ALU.mult, ALU.add, [("xs", hh), ("SCB",), ("ps", 3)], [("xs", hh)])
                    cp(XB[p0:p0 + 64, dr, :], XS[p0:p0 + 64, dr, :], [("xs", hh)], [("xb", hh)], eng="act")
                for hh in range(2):
                    cp(DN[:, hh:hh + 1], PS[OB[hh]][:, 128:129], [("ps", OB[hh])], [("dn",)])
                r0 = dr * 4 + 2 * u
                stt(DD, DN, -1.0, DN, ALU.mult, ALU.max, [("dn",)], [("dd",)])
                tt(DD, DD, KWT[:, c, 2, r0:r0 + 2], ALU.max, [("dd",), ("KWT", c)], [("dd",)])
                recip(DD, DD, [("dd",)], [("dd",)])
                for hh in range(2):
                    hs = slice(hh * 128, (hh + 1) * 128)
                    if dr == 0:
                        act(HF[:, c, hs], PS[OB[hh]][:, 0:128], AF.Copy, [("ps", OB[hh]), ("dd",)], [("hf", c)], scale=DD[:, hh:hh + 1])
                    else:
                        stt(HS[:, hs], PS[OB[hh]][:, 0:128], DD[:, hh:hh + 1], HF[:, c, hs], ALU.mult, ALU.add,
                            [("ps", OB[hh]), ("dd",), ("hf", c)], [("hs",)])
                if dr == 1:
                    for hh in range(2):
                        P.add("act", lambda e, hh=hh: e.activation(out=HQ[:, hh * 128:(hh + 1) * 128], in_=HS[:, hh * 128:(hh + 1) * 128],
                                                                   func=AF.Square, accum_out=SS[:, hh:hh + 1]),
                              [("hs",), ("hq",), ("ss",)], [("hq",), ("ss",)])
                    act(SS, SS, AF.Ln, [("ss",)], [("ss",)], scale=1.0 / 128, bias=EPS)
                    act(SS, SS, AF.Exp, [("ss",)], [("ss",)], scale=-0.5)
                    tt(HS.rearrange("p (h c) -> p h c", h=2), HS.rearrange("p (h c) -> p h c", h=2), SS.unsqueeze(2).to_broadcast([128, 2, 128]),
                       ALU.mult, [("hs",), ("ss",)], [("hs",)])
                    tt(HS, HS, MLG[:, u * 256:(u + 1) * 256], ALU.mult, [("hs",), ("MLG",)], [("hs",)])
                    tt(HMb, HS, SIG[:, c, :], ALU.mult, [("hs",), ("sig", c)], [("hmb",)])
                    def tr_out(c=c, csl=csl):
                        for hh in range(2):
                            tr(PSB[:, hh * 128:(hh + 1) * 128], HMb[:, hh * 128:(hh + 1) * 128], identb, [("hmb",), ("CSTB",)], [("psb",)])
                        for hh in range(2):
                            cp(MIXv[:, 4 + 2 * u + hh, csl], PSB[:, hh * 128:(hh + 1) * 128], [("psb",)], [("MIXm", 2 * u + hh, c)], eng="act")
                    pend_tr.append(tr_out)

    for u in range(2):
        ml_unit(u)
        if stop in ("ml1", "mlA", "mlB", "mlC"):
            break
    if stop == "mlC":
        tap(HF[:, 0, :], 256, [("hf", 0)])
        tap(HF[:, 1, :], 256, [("hf", 1)])
        return finish()
    if stop in ("mlA", "mlB"):
        return finish()
    if stop in ("ml", "ml1"):
        for j in range(4, 8):
            tap(MIXv[:, j, :], T, [("MIXm", j - 4, c) for c in range(NT)])
        return finish()

    def out_proj(l, w_out, xold_view, tiles, mix_keys, xold_keys=lambda j: []):
        fence("wo%d" % l)
        WO = SCRb[:, 0:8192].rearrange("p (k c) -> p k c", k=KC)
        WOS = SCR[:, 4096:6144].rearrange("p (k c) -> p k c", k=KC)
        XO = SCR[:, 6144:8192].rearrange("p (s t) -> p s t", s=4)
        wv = w_out.rearrange("(k p) c -> p k c", p=128)
        for pc in range(4):
            dma(WOS, wv[:, :, pc * 256:(pc + 1) * 256], [], [("wos",)])
            cp(WO[:, :, pc * 256:(pc + 1) * 256], WOS, [("wos",)], [("wo", pc)], eng="pool")
        for i in tiles:
            t0, n = TT[i]
            seg = 1 if i == 0 else 0
            for j in range(KC):
                pi = cnt["pj"] % 2
                cnt["pj"] += 1
                for kc in range(KC):
                    mm(PS[pi][:, 0:n], WO[:, kc, j * 128:(j + 1) * 128], MIXv[:, kc, t0:t0 + n], kc == 0, kc == KC - 1,
                       [("wo", j // 2)] + mix_keys(i), [("ps", pi)])
                xi = cnt["bt"] % 4
                cnt["bt"] += 1
                dma(XO[:, xi, 0:n], xold_view[:, j, t0:t0 + n], xold_keys(j), [("xo", xi)])
                stt(X[:, j, t0:t0 + n], PS[pi][:, 0:n], MOD[:, l, 16 + j, seg:seg + 1], XO[:, xi, 0:n], ALU.mult, ALU.add,
                    [("ps", pi), ("MOD", l), ("xo", xi)], [("X", i, j)])

    def mix_keys0(i):
        t0, n = TT[i]
        return [("MIX", u, i) for u in range(4)] + [("MIXm", jj, c) for jj in range(4) for c in range(t0 // 128, (t0 + n) // 128)]

    out_proj(0, w_out0, xv, range(5), mix_keys0)
    if stop == "wo0":
        for j in range(KC):
            tap(X[:, j, :], T, [("X", i, j) for i in range(5)])
        return finish()

    RW = sb("RW", [128, KC, NE], F32)
    dma(RW[:], router_w.rearrange("(k p) e -> p k e", p=128), [], [("RW",)])
    COMB = sb("COMB", [128, NT, NE], F32)
    WX = sb("WX", [128, 6144], BF16)

    def moe(l, tiles):
        fence("moe%d" % l)
        nt0 = TT[tiles[0]][0] // 128
        ntb = NT - nt0
        H32 = MIX[:, 0:8192].bitcast(F32).rearrange("p (k t) -> p k t", k=KC)
        LGS = SML[:, 64:64 + 0]

        def h32_cb(i, sq, s2, t0, n):
            seg = 1 if i == 0 else 0
            for kc in range(KC):
                ts(H32[:, kc, 0:n], sq[:, kc, :], GS[:, l, 1, kc, seg:seg + 1], MOD[:, l, 24 + kc, seg:seg + 1], ALU.mult, ALU.add,
                   [("sq", s2), ("GS", l, 1), ("MOD", l)], [("h32",)])
            for b in range(n // 128):
                tb = t0 // 128 + b
                for kc in range(KC):
                    mm(PS[6][:, tb * NE:(tb + 1) * NE], H32[:, kc, b * 128:(b + 1) * 128], RW[:, kc, :], kc == 0, kc == KC - 1,
                       [("h32",), ("RW",)], [("ps", 6)])

        def src_x(i):
            t0, n = TT[i]
            return X[:, :, t0:t0 + n], [("X", i, j) for j in range(KC)]

        norm_modulate(l, 1, src_x, tiles, h32_cb)
        RT = SCR[:, 0:4608]
        SC_ = RT[:, 0:288].rearrange("p (t e) -> p t e", e=NE)
        SEL = RT[:, 288:576].rearrange("p (t e) -> p t e", e=NE)
        EQ = RT[:, 576:864].rearrange("p (t e) -> p t e", e=NE)
        SEL2 = RT[:, 864:1152].rearrange("p (t e) -> p t e", e=NE)
        M1 = RT[:, 1152:1224]
        M2 = RT[:, 1224:1296]
        GSC = RT[:, 1296:1368]
        GMX = RT[:, 1368:1386]
        GMK = RT[:, 1386:1458]
        WS_ = RT[:, 1458:1476]
        g4 = lambda a: a.rearrange("p t (g j) -> p (t g) j", j=4)
        act(SC_, PS[6][:, 0:288].rearrange("p (t e) -> p t e", e=NE), AF.Sigmoid, [("ps", 6)], [("r_sc",)])
        tt(SEL, SC_, SML[:, 16:32].unsqueeze(1).to_broadcast([128, NT, NE]), ALU.add, [("r_sc",), ("router_b",)], [("r_sel",)])
        red(M1, g4(SEL), ALU.max, [("r_sel",)], [("r_m1",)])
        tt(g4(EQ), g4(SEL), M1.unsqueeze(2).to_broadcast([128, 72, 4]), ALU.is_equal, [("r_sel",), ("r_m1",)], [("r_eq",)])
        stt(SEL2, EQ, -1e9, SEL, ALU.mult, ALU.add, [("r_eq",), ("r_sel",)], [("r_sel2",)])
        red(M2, g4(SEL2), ALU.max, [("r_sel2",)], [("r_m2",)])
        tt(GSC, M1, M2, ALU.add, [("r_m1",), ("r_m2",)], [("r_gsc",)])
        red(GMX, GSC.rearrange("p (t g) -> p t g", g=4), ALU.max, [("r_gsc",)], [("r_gmx",)])
        tt(GMK.rearrange("p (t g) -> p t g", g=4), GSC.rearrange("p (t g) -> p t g", g=4), GMX.unsqueeze(2).to_broadcast([128, NT, 4]),
           ALU.is_equal, [("r_gsc",), ("r_gmx",)], [("r_gmk",)])
        tt(g4(EQ), g4(SEL), M2.unsqueeze(2).to_broadcast([128, 72, 4]), ALU.is_ge, [("r_sel",), ("r_m2",), ("r_eq",)], [("r_eq",)])
        tt(g4(EQ), g4(EQ), GMK.unsqueeze(2).to_broadcast([128, 72, 4]), ALU.mult, [("r_eq",), ("r_gmk",)], [("r_eq",)])
        tt(SEL2, SC_, EQ, ALU.mult, [("r_sc",), ("r_eq",), ("r_sel2",)], [("r_sel2",)])
        red(WS_, SEL2, ALU.add, [("r_sel2",)], [("r_ws",)])
        recip(WS_, WS_, [("r_ws",)], [("r_ws",)])
        tt(COMB[:], SEL2, WS_.unsqueeze(2).to_broadcast([128, NT, NE]), ALU.mult, [("r_sel2",), ("r_ws",)], [("COMB",)])
        if stop == "route%d" % l:
            tap(COMB[:].rearrange("p t e -> p (t e)"), 288, [("COMB",)])
            return True
        fence("moe%de" % l)
        MIXb = MIX[:]
        HSL = []
        for sl in range(4):
            HSL.append((MIXb[:, sl * 4096:sl * 4096 + 2048], MIXb[:, sl * 4096 + 2048:(sl + 1) * 4096]))
        HSL.append((WX[:, 0:2048], WX[:, 2048:4096]))
        HSL.append((WX[:, 4096:6144], MIXb[:, 16384:18432]))
        STG = SCR[:, 0:4096].rearrange("p (s c) -> p s c", s=2)
        GB = SCRb[:, 8192:12288].rearrange("p (s f t) -> p s f t", s=2, f=4)
        AR = SCRb[:, 12288:13312].rearrange("p (s t) -> p s t", s=2)
        TM = SCRb[:, 13312:14336].rearrange("p (s t) -> p s t", s=2)
        CB = SCRb[:, 14336:15360].rearrange("p (s t) -> p s t", s=2)
        DG = SCRb[:, 15360:15872]
        mcnt = [0, 0]

        n_exp = int(os.environ.get("NEXP", NE))
        halves = []
        WTAB = []
        for e in range(n_exp):
            mats = []
            for mi, (dram2d, kparts, ncols) in enumerate(((exp_w1[l, e], 8, DFF), (exp_w3[l, e], 8, DFF), (exp_w2[l, e], 4, D))):
                sl = (3 * e + mi) % 6
                src = dram2d.rearrange("(k p) c -> p k c", p=128)
                half = kparts // 2
                out = []
                for hf in range(2):
                    dst = HSL[sl][hf].rearrange("p (k c) -> p k c", k=half)
                    key = ("wsl", sl, hf)
                    halves.append((src[:, hf * half:(hf + 1) * half, :], dst, key, half))
                    out.append((dst, key))
                mats.append(out)
            WTAB.append(mats)
        ptr = [0, 0]

        def emit_dma():
            q = ptr[0]
            if q >= len(halves):
                return
            ptr[0] += 1
            src, dst, key, half = halves[q]
            st = STG[:, q % 2, :].rearrange("p (k c) -> p k c", k=half)
            dma(st, src, [], [("stg", q % 2)])

        def advance():
            q = ptr[1]
            if q >= len(halves):
                return
            ptr[1] += 1
            src, dst, key, half = halves[q]
            st = STG[:, q % 2, :].rearrange("p (k c) -> p k c", k=half)
            cp(dst, st, [("stg", q % 2)], [key], eng="act")
            emit_dma()

        emit_dma()
        emit_dma()
        for _ in range(6):
            advance()

        DG2 = SCRb[:, 15360:16384].rearrange("p (s t) -> p s t", s=2)

        def prep_dg(k):
            if k >= len(seq):
                return
            e, i = seq[k]
            t0, n = TT[i]
            tb0, nb = t0 // 128, n // 128
            tt(DG2[:, k % 2, 0:n].rearrange("p (b t) -> p b t", t=128), identb.unsqueeze(1).to_broadcast([128, nb, 128]),
               COMB[:, tb0:tb0 + nb, e:e + 1].to_broadcast([128, nb, 128]), ALU.mult, [("CSTB",), ("COMB",)], [("dg", k % 2)])

        def prep_cb(k):
            if k >= len(seq):
                return
            e, i = seq[k]
            t0, n = TT[i]
            mm(PS[6][:, 0:n], onesb, DG2[:, k % 2, 0:n], True, True, [("dg", k % 2), ("CSTB",)], [("ps", 6)])
            cp(CB[:, k % 2, 0:n], PS[6][:, 0:n], [("ps", 6)], [("cb", k % 2)], eng="act")

        def h_phase(k, W1, W3):
            e, i = seq[k]
            t0, n = TT[i]
            ci = k % 2
            gi = cnt["po"] % 2
            cnt["po"] += 1
            for fc in range(4):
                pa = cnt["pj"] % 2
                cnt["pj"] += 1
                pb = 2 + pa
                for kc in range(KC):
                    w, wk = W1[kc // 4]
                    mm(PS[pa][:, 0:n], w[:, kc % 4, fc * 128:(fc + 1) * 128], HT[:, kc, t0:t0 + n], kc == 0, kc == KC - 1,
                       [wk] + bkeys("HT", t0, n), [("ps", pa)])
                for kc in range(KC):
                    w, wk = W3[kc // 4]
                    mm(PS[pb][:, 0:n], w[:, kc % 4, fc * 128:(fc + 1) * 128], HT[:, kc, t0:t0 + n], kc == 0, kc == KC - 1,
                       [wk] + bkeys("HT", t0, n), [("ps", pb)])
                if fc == 2:
                    prep_cb(k + 1)
                ai = cnt["pt"] % 2
                cnt["pt"] += 1
                act(AR[:, ai, 0:n], PS[pa][:, 0:n], AF.Silu, [("ps", pa)], [("ar", ai)])
                tt(TM[:, ai, 0:n], PS[pb][:, 0:n], CB[:, ci, 0:n], ALU.mult, [("ps", pb), ("cb", ci)], [("tm", ai)])
                tt(GB[:, gi, fc, 0:n], AR[:, ai, 0:n], TM[:, ai, 0:n], ALU.mult, [("ar", ai), ("tm", ai)], [("gb", gi, fc)], eng="pool")
            return gi

        def y_phase(i, gi, W2):
            t0, n = TT[i]
            seg = 1 if i == 0 else 0
            for j in range(KC):
                py = 4 + cnt["st"] % 2
                cnt["st"] += 1
                for fc in range(4):
                    w, wk = W2[fc // 2]
                    mm(PS[py][:, 0:n], w[:, fc % 2, j * 128:(j + 1) * 128], GB[:, gi, fc, 0:n], fc == 0, fc == 3,
                       [wk, ("gb", gi, fc)], [("ps", py)])
                stt(X[:, j, t0:t0 + n], PS[py][:, 0:n], MOD[:, l, 40 + j, seg:seg + 1], X[:, j, t0:t0 + n], ALU.mult, ALU.add,
                    [("ps", py), ("MOD", l), ("X", i, j)], [("X", i, j)])

        pend = None
        tl = list(tiles)
        seq = [(e, i) for e in range(n_exp) for i in tl]
        sched = [2, 1, 1, 1, 1] if len(tl) == 5 else [2, 1, 2, 1]
        prep_dg(0)
        prep_cb(0)
        prep_dg(1)
        for k, (e, i) in enumerate(seq):
            W1, W3, W2 = WTAB[e]
            gi = h_phase(k, W1, W3)
            prep_dg(k + 2)
            if pend is not None:
                y_phase(*pend)
            pend = (i, gi, W2)
            for _ in range(sched[k % len(tl)]):
                advance()
        y_phase(*pend)
        return False

    if moe(0, range(5)):
        return finish()
    if stop == "l0":
        for j in range(KC):
            tap(X[:, j, :], T, [("X", i, j) for i in range(5)])
        return finish()

    def src_x1(i):
        t0, n = TT[i]
        return X[:, :, t0:t0 + n], [("X", i, j) for j in range(KC)]

    norm_modulate(1, 0, src_x1, range(5))
    xspv = xsp.rearrange("(k p) t -> p k t", p=128)
    for j in range(KC):
        dma(xspv[:, j, 256:T], X[:, j, 256:T], [("X", i, j) for i in range(1, 5)], [("xsp", j)])
    fence("gqa")
    wv1 = w_in1.rearrange("(k p) c -> p k c", p=128)
    gkT = XRb[:, 0:4608].rearrange("p (v t) -> p v t", v=2)
    gV = XRb[:, 4608:9216].rearrange("p (t v c) -> p t v c", t=NT, v=2)
    gQ = [XRb[:, 9216:11264], XRb[:, 11264:13312]]
    RPC = XR[:, 6656:8704]
    RPS = XR[:, 8704:10752]
    GW = XRb[:, 21504:25600].rearrange("p (s k c) -> p s k c", s=2, k=KC)
    GWS = XR[:, 12800:14848].rearrange("p (k c) -> p k c", k=KC)
    GPT = XRb[:, 29696:31744].rearrange("p (s t) -> p s t", s=4)
    QN = XRb[:, 31744:32256]
    TMPA = XR[:, 16128:16640]
    TMPB = XR[:, 16640:17152]
    GRC = XR[:, 17152:17664]
    GRS = XR[:, 17664:18176]
    GSQ = XRb[:, 36352:36864]
    GQG = SML[:, 10:12]
    dma(RPC, ropeC[:, :], [], [("rpc",)])
    dma(RPS, ropeS[:, :], [], [("rps",)])
    ts(GQG[:, 0:1], SML[:, 4:5], 128.0 ** -0.5, None, ALU.mult, None, [("gqa_g",)], [("gqg",)])
    cp(GQG[:, 1:2], SML[:, 5:6], [("gqa_g",), ("gqg",)], [("gqg",)])
    gwc = [0]

    def load_w1(c0, ncol):
        si = gwc[0] % 2
        gwc[0] += 1
        dma(GWS[:, :, 0:ncol], wv1[:, :, c0:c0 + ncol], [], [("gws",)])
        cp(GW[:, si, :, 0:ncol], GWS[:, :, 0:ncol], [("gws",)], [("gw", si)], eng="pool")
        return GW[:, si], ("gw", si)

    def qk_norm_rope(ps, pi, n, gcol, dst, rope_t0, wkeys, stat=None):
        sps, skey = (PS[6], ("ps", 6)) if stat is None else (stat[0], (stat[1],))
        act(GSQ[:, 0:n], ps[:, 0:n], AF.Square, [("ps", pi)], [("gsq",)])
        mm(sps[:, 0:n], onesb, GSQ[:, 0:n], True, True, [("gsq",), ("CSTB",)], [skey])
        act(GRS[:, 0:n], sps[:, 0:n], AF.Ln, [skey], [("grs",)], scale=1.0 / 128, bias=EPS)
        act(GRS[:, 0:n], GRS[:, 0:n], AF.Exp, [("grs",)], [("grs",)], scale=-0.5)
        if rope_t0 is None:
            stt(dst, ps[:, 0:n], GQG[:, gcol:gcol + 1], GRS[:, 0:n], ALU.mult, ALU.mult, [("ps", pi), ("grs",), ("gqg",)], wkeys)
            return
        stt(QN[:, 0:n], ps[:, 0:n], GQG[:, gcol:gcol + 1], GRS[:, 0:n], ALU.mult, ALU.mult, [("ps", pi), ("grs",), ("gqg",)], [("qn",)])
        mm(sps[:, 0:n], swapb, QN[:, 0:n], True, True, [("qn",), ("CSTB",)], [skey])
        tt(TMPA[:, 0:n], sps[:, 0:n], RPS[:, rope_t0:rope_t0 + n], ALU.mult, [skey, ("rps",)], [("tmpa",)])
        tt(TMPB[:, 0:n], QN[:, 0:n], RPC[:, rope_t0:rope_t0 + n], ALU.mult, [("qn",), ("rpc",)], [("tmpb",)])
        tt(dst, TMPA[:, 0:n], TMPB[:, 0:n], ALU.add, [("tmpa",), ("tmpb",)], wkeys)

    Wk, wkk = load_w1(1024, 256)
    for kv in range(2):
        for i in range(5):
            t0, n = TT[i]
            pi = cnt["pj"] % 2
            cnt["pj"] += 1
            for kc in range(KC):
                mm(PS[pi][:, 0:n], Wk[:, kc, kv * 128:(kv + 1) * 128], HT[:, kc, t0:t0 + n], kc == 0, kc == KC - 1,
                   [wkk] + bkeys("HT", t0, n), [("ps", pi)])
            qk_norm_rope(PS[pi], pi, n, 1, gkT[:, kv, t0:t0 + n], None if i == 0 else t0 - 256, [("gk", kv, i)])
    Wv, wvk = load_w1(1280, 256)
    for g in range(9):
        pi = cnt["pj"] % 2
        cnt["pj"] += 1
        for b2 in range(2):
            tb = g * 2 + b2
            for kc in range(KC):
                mm(PS[pi][:, b2 * 256:(b2 + 1) * 256], HT[:, kc, tb * 128:(tb + 1) * 128], Wv[:, kc, :], kc == 0, kc == KC - 1,
                   [wvk, ("HT", tb)], [("ps", pi)])
        cp(gV[:, g * 2:g * 2 + 2].rearrange("p t v c -> p t (v c)"), PS[pi][:, 0:512].rearrange("p (t c) -> p t c", t=2),
           [("ps", pi)], [("gv", g)], eng="act")
    if stop == "gqakv":
        tap(gkT[:, 0, :], T, [("gk", 0, i) for i in range(5)])
        tap(gkT[:, 1, :], T, [("gk", 1, i) for i in range(5)])
        return finish()

    PSBf = PSB[:].bitcast(F32)

    def q_stages(h):
        st = []
        qT = gQ[h % 2]
        hold = {}

        def s_load():
            hold["w"] = load_w1(h * 128, 128)
        st.append(s_load)
        for i in range(1, 5):
            t0, n = TT[i]
            r0 = t0 - 256
            dst = qT[:, r0:r0 + n]

            def s0(t0=t0, n=n):
                Wq, wqk = hold["w"]
                for kc in range(KC):
                    mm(PS[6][:, 0:n], Wq[:, kc, 0:128], HT[:, kc, t0:t0 + n], kc == 0, kc == KC - 1, [wqk] + bkeys("HT", t0, n), [("ps", 6)])

            def s1(n=n):
                act(GSQ[:, 0:n], PS[6][:, 0:n], AF.Square, [("ps", 6)], [("gsq",)])

            def s2(n=n):
                mm(PSBf[:, 0:n], onesb, GSQ[:, 0:n], True, True, [("gsq",), ("CSTB",)], [("psb",)])

            def s3(n=n):
                act(GRS[:, 0:n], PSBf[:, 0:n], AF.Ln, [("psb",)], [("grs",)], scale=1.0 / 128, bias=EPS)
                act(GRS[:, 0:n], GRS[:, 0:n], AF.Exp, [("grs",)], [("grs",)], scale=-0.5)

            def s4(n=n):
                stt(QN[:, 0:n], PS[6][:, 0:n], GQG[:, 0:1], GRS[:, 0:n], ALU.mult, ALU.mult, [("ps", 6), ("grs",), ("gqg",)], [("qn",)])

            def s5(n=n):
                mm(PSBf[:, 0:n], swapb, QN[:, 0:n], True, True, [("qn",), ("CSTB",)], [("psb",)])

            def s6(n=n, r0=r0):
                tt(TMPA[:, 0:n], PSBf[:, 0:n], RPS[:, r0:r0 + n], ALU.mult, [("psb",), ("rps",)], [("tmpa",)])
                tt(TMPB[:, 0:n], QN[:, 0:n], RPC[:, r0:r0 + n], ALU.mult, [("qn",), ("rpc",)], [("tmpb",)])

            def s7(n=n, dst=dst, i=i):
                tt(dst, TMPA[:, 0:n], TMPB[:, 0:n], ALU.add, [("tmpa",), ("tmpb",)], [("gq", h % 2, i)])

            st += [s0, s1, s2, s3, s4, s5, s6, s7]
        return st

    def q_proj(h):
        for f in q_stages(h):
            f()

    iters = [(h, m, kt) for h in range(8) for m in range(4) for kt in range(NT)]

    STB = [PS[0], PS[1], PS[5]]
    STK = [("ps", 0), ("ps", 1), ("ps", 5)]
    ACC = SCR[:, 0:2048].rearrange("p (s e t) -> p s e t", s=2, e=2)

    def emit_st(idx):
        if idx >= len(iters):
            return
        h, m, kt = iters[idx]
        si = idx % 3
        kti = 0 if kt < 2 else 1 + (kt - 2) // 4
        mm(STB[si][:, 0:512], gkT[:, h // 4, kt * 128:(kt + 1) * 128], gQ[h % 2][:, m * 512:(m + 1) * 512], True, True,
           [("gk", h // 4, kti), ("gq", h % 2, m + 1)], [STK[si]])

    q_proj(0)
    emit_st(0)
    emit_st(1)
    for idx, (h, m, kt) in enumerate(iters):
        if stop == "gqa1" and idx >= NT:
            break
        kv = h // 4
        si = idx % 3
        blk = idx // NT
        oi = 2 + blk % 2
        ai = blk % 2
        emit_st(idx + 2)
        pti = idx % 4
        act(GPT[:, pti, :], STB[si][:, 0:512], AF.Exp, [STK[si]], [("gpt", pti)])
        mm(PS[oi][:, 0:512], gV[:, kt, kv, :], GPT[:, pti, :], kt == 0, kt == NT - 1, [("gv", kt // 2), ("gpt", pti)], [("ps", oi)])
        ae = kt % 2
        aeng = "dve" if ae == 0 else "pool"
        if kt < 2:
            cp(ACC[:, ai, ae, :], GPT[:, pti, :], [("gpt", pti)], [("acc", ai, ae)], eng=aeng)
        else:
            tt(ACC[:, ai, ae, :], ACC[:, ai, ae, :], GPT[:, pti, :], ALU.add, [("acc", ai, ae), ("gpt", pti)], [("acc", ai, ae)], eng=aeng)
        if kt == NT - 1:
            mm(PS[4][:, 0:512], ones_f, ACC[:, ai, 0, :], True, False, [("CST",), ("acc", ai, 0)], [("ps", 4)])
            mm(PS[4][:, 0:512], ones_f, ACC[:, ai, 1, :], False, True, [("CST",), ("acc", ai, 1)], [("ps", 4)])
            recip(GRC, PS[4][:, 0:512], [("ps", 4)], [("grc",)])
            tt(MIXv[:, h, 256 + m * 512:256 + (m + 1) * 512], PS[oi][:, 0:512], GRC, ALU.mult, [("ps", oi), ("grc",)], [("MIXg", h, m + 1)])
        if m == 0 and kt == 0:
            pendq = q_stages(h + 1) if h + 1 < 8 else []
        if pendq and idx % 2 == 1:
            pendq.pop(0)()
    if stop == "gqa1":
        tap(MIXv[:, 0, 256:768], 512, [("MIXg", 0, 1)])
        return finish()
    if stop == "gqa":
        for h in range(8):
            tap(MIXv[:, h, 256:T], S_LAT, [("MIXg", h, m) for m in range(1, 5)])
        return finish()

    out_proj(1, w_out1, xspv, range(1, 5), lambda i: [("MIXg", h, i) for h in range(8)], lambda j: [("xsp", j)])
    if stop == "wo1":
        for j in range(KC):
            tap(X[:, j, 256:T], S_LAT, [("X", i, j) for i in range(1, 5)])
        return finish()
    if moe(1, range(1, 5)):
        return finish()
    ov = outT.rearrange("(k p) t -> p k t", p=128)
    for j in range(KC):
        dma(ov[:, j, :], X[:, j, 256:T], [("X", i, j) for i in range(1, 5)], [("out", j)])
        k_.out_keys.append(("out", j))
    return finish()


def _fm(v):
    v = np.asarray(v, np.float32)
    return np.ascontiguousarray(v.reshape(-1, 128).T)


def _const_tables():
    c = np.zeros((128, 1024), np.float32)
    c[:, 0:128] = np.eye(128, dtype=np.float32)
    c[:, 128:256] = 1.0
    j = np.arange(128)[:, None]
    i = np.arange(128)[None, :]
    c[:, 256:384] = (j <= i)
    c[:, 384:512] = (j >= i)
    sw = np.zeros((128, 128), np.float32)
    sw[(np.arange(128) + 64) % 128, np.arange(128)] = 1.0
    c[:, 512:640] = sw
    c[:, 640:768] = 1.0
    bd = np.zeros((128, 128), np.float32)
    bd[0:64, 0:64] = 1.0
    bd[64:128, 64:128] = 1.0
    c[:, 768:896] = bd
    for r in range(8):
        c[(r if r < 4 else 32 + r - 4), 896 + r] = 1.0
    return c


def _rope_tables():
    n_freq = 128 // 4
    inv_freq = (np.float32(10000.0) ** (-np.arange(n_freq, dtype=np.float32) / np.float32(n_freq))).astype(np.float32)
    t = np.arange(S_LAT)
    rows = (t // 64).astype(np.float32)
    cols = (t % 64).astype(np.float32)
    ang = np.concatenate([rows[:, None] * inv_freq, cols[:, None] * inv_freq], axis=-1).astype(np.float32)
    cos = np.cos(ang).astype(np.float32).T
    sin = np.sin(ang).astype(np.float32).T
    C = np.concatenate([cos, cos], 0)
    S = np.concatenate([-sin, sin], 0)
    return np.ascontiguousarray(C), np.ascontiguousarray(S)


def na_tile_index(m, j):
    if m == 0:
        return j
    if m == 3:
        return 14 + (j - 10)
    return 6 + (j - (4 * m - 2))


def na_key_tiles(m):
    if m == 0:
        return list(range(0, 6))
    if m == 3:
        return list(range(10, 16))
    return list(range(4 * m - 2, 4 * m + 6))


def _na_bias_tables(rpb):
    rpb = np.asarray(rpb, np.float32)
    out = np.full((8, 20, 128, 512), NEG, np.float32)
    kc = np.arange(64)
    qc = np.arange(64)
    c0 = np.clip(qc - 8, 0, 48)
    colok = (kc[:, None] >= c0[None, :]) & (kc[:, None] < c0[None, :] + 16)
    dcol = np.clip(kc[:, None] - qc[None, :] + 15, 0, 30)
    for m in (0, 1, 3):
        for j in na_key_tiles(m):
            ti = na_tile_index(m, j)
            for krl in range(2):
                kr = 2 * j + krl
                for qrl in range(8):
                    qr = 8 * m + qrl
                    r0 = min(max(qr - 4, 0), 24)
                    if not (r0 <= kr <= r0 + 7):
                        continue
                    drow = kr - qr + 7
                    blk = rpb[:, drow][:, dcol]
                    blk = np.where(colok[None], blk, np.float32(NEG))
                    out[:, ti, krl * 64:(krl + 1) * 64, qrl * 64:(qrl + 1) * 64] = blk
    return out


def prepare_inputs(inputs):
    f = lambda k: np.asarray(inputs[k], np.float32)
    sh = {}
    sh["ada_w"] = f("ada_w")
    sh["adab"] = np.ascontiguousarray(f("ada_b").reshape(2, 48, 128).transpose(2, 0, 1))
    nm = f("norm_mix_g").reshape(2, KC, 128)
    nf = f("norm_ffn_g").reshape(2, KC, 128)
    sh["nrm_g"] = np.ascontiguousarray(np.stack([nm, nf], 1).transpose(3, 0, 1, 2))
    w_in0 = f("even_w_in")[0]
    sh["w_in0"] = w_in0
    sh["w_out0"] = f("even_w_out")[0]
    wg = np.zeros((D, 128), np.float32)
    g = w_in0[:, 3072:3088]
    wg[:, 0:4] = g[:, 0:4]
    wg[:, 32:36] = g[:, 8:12]
    wg[:, 64:68] = g[:, 4:8]
    wg[:, 96:100] = g[:, 12:16]
    sh["wgate"] = wg
    gb = f("mlstm_gate_b")[0]
    gateb = np.zeros((128, 2), np.float32)
    gateb[0:4, 0] = gb[0:4]
    gateb[32:36, 0] = gb[8:12]
    gateb[0:4, 1] = gb[4:8]
    gateb[32:36, 1] = gb[12:16]
    sh["gateb"] = gateb
    sh["na_g"] = np.ascontiguousarray(np.stack([np.tile(f("na_q_norm_g")[0], 2), np.tile(f("na_k_norm_g")[0], 2)], 1))
    sh["btab"] = _na_bias_tables(f("na_rpb")[0])
    sh["ml_g"] = np.ascontiguousarray(np.broadcast_to(f("mlstm_norm_g")[0][None, :], (128, 512)))
    perm = np.concatenate([np.arange(0, 128, 2), np.arange(1, 128, 2)])
    w_in1 = f("odd_w_in")[0].copy()
    for h in range(10):
        w_in1[:, h * 128:(h + 1) * 128] = w_in1[:, h * 128:(h + 1) * 128][:, perm]
    sh["w_in1"] = w_in1
    sh["w_out1"] = f("odd_w_out")[0]
    sh["gqa_g"] = np.ascontiguousarray(np.stack([f("gqa_q_norm_g")[0][perm], f("gqa_k_norm_g")[0][perm]], 1))
    C, S = _rope_tables()
    sh["ropeC"], sh["ropeS"] = C, S
    sh["router_w"] = f("router_w")
    sh["router_b"] = np.ascontiguousarray(np.broadcast_to(f("router_b")[None, :], (128, NE)))
    sh["exp_w1"], sh["exp_w3"], sh["exp_w2"] = f("exp_w1"), f("exp_w3"), f("exp_w2")
    sh["consts"] = _const_tables()
    x, ctx, c, c_ctx = f("x"), f("ctx"), f("c"), f("c_ctx")
    per = []
    for b in range(x.shape[0]):
        d = dict(sh)
        d["xT"] = np.ascontiguousarray(np.concatenate([ctx[b], x[b]], 0).T)
        d["cvec"] = np.ascontiguousarray(np.stack([_fm(c[b]), _fm(c_ctx)], 2))
        per.append(d)
    return per


_CACHE = {}


def kernel(**inputs):
    per = prepare_inputs(inputs)
    if "nc" not in _CACHE:
        _CACHE["nc"] = build_program()[0]
    nc = _CACHE["nc"]
    res = run_bass_kernel_spmd(nc, per, core_ids=list(range(len(per))))
    out = np.stack([np.ascontiguousarray(r["outT"].T) for r in res.results], 0)
    return out.astype(np.float32)
```
